# Optimizing a Trainium2 kernel written in Bass

```python
import math
import jax, jax.numpy as jnp
from jax import lax
import numpy as np

D_MODEL = 1024
BATCH = 8
SEQ = 4096
DEPTH = 2

MEM_LEN = 256
D_MIX = D_MODEL
HEAD_DIM = 64
ATTN_Q_HEADS = 8
ATTN_KV_HEADS = 2
ATTN_WIDTH = ATTN_Q_HEADS * HEAD_DIM
KV_WIDTH = ATTN_KV_HEADS * HEAD_DIM
WINDOW = 128
BLOCK = 128
SC_WIDTH = D_MIX // 4
SC_CONV = 3
RG_HEADS = 4
RG_WIDTH = D_MIX // 4
RG_HEAD_DIM = RG_WIDTH // RG_HEADS
RG_CONV = 4
RG_C = 8.0
N_BUCKETS = 32
MAX_EXACT = N_BUCKETS // 2
MAX_DISTANCE = 128
XA_HEADS = 4
XA_HEAD_DIM = 128
XA_WIDTH = XA_HEADS * XA_HEAD_DIM
D_FF_DENSE = 2816
N_EXPERTS = 8
TOP_K = 2
D_FF_EXPERT = 3584
N_DENSE = (DEPTH + 1) // 2
N_MOE = DEPTH // 2
EPS = 1e-6
NEG_INF = -1e30
IN_COLS = ATTN_WIDTH + 2 * KV_WIDTH + 3 * SC_WIDTH + 2 * RG_WIDTH

kernel_name = "hymba_style_hybrid_swa_shortconv_rglru_moe"


def rms_norm(x, g):
    xf = x.astype(jnp.float32)
    y = xf * lax.rsqrt(jnp.mean(xf * xf, axis=-1, keepdims=True) + EPS) * g.astype(jnp.float32)
    return y.astype(x.dtype)


def causal_dwconv(u, w, b):
    k_width = w.shape[0]
    s = u.shape[1]
    up = jnp.pad(u, ((0, 0), (k_width - 1, 0), (0, 0)))
    out = b
    for k in range(k_width):
        out = out + w[k] * up[:, k:k + s]
    return out


def t5_bucket(dist):
    n = jnp.maximum(dist, 0)
    large = MAX_EXACT + (jnp.log(jnp.maximum(n, 1).astype(jnp.float32) / MAX_EXACT)
                         / math.log(MAX_DISTANCE / MAX_EXACT) * (N_BUCKETS - MAX_EXACT)).astype(jnp.int32)
    large = jnp.minimum(large, N_BUCKETS - 1)
    return jnp.where(n < MAX_EXACT, n, large)


def sliding_window_sink_attention(q, k, v, sinks, rel_bias):
    b, s, _ = q.shape
    nb = s // BLOCK
    g = ATTN_Q_HEADS // ATTN_KV_HEADS
    qb = q.reshape(b, nb, BLOCK, ATTN_KV_HEADS, g, HEAD_DIM)
    kb = k.reshape(b, nb, BLOCK, ATTN_KV_HEADS, HEAD_DIM)
    vb = v.reshape(b, nb, BLOCK, ATTN_KV_HEADS, HEAD_DIM)

    def band(t):
        prev = jnp.pad(t, ((0, 0), (1, 0), (0, 0), (0, 0), (0, 0)))[:, :-1]
        return jnp.concatenate([prev, t], axis=2)

    kband, vband = band(kb), band(vb)
    scores = jnp.einsum('bnqhgd,bnkhd->bnhgqk', qb, kband,
                        preferred_element_type=jnp.float32) / math.sqrt(HEAD_DIM)
    q_idx = jnp.arange(BLOCK)
    k_idx = jnp.arange(2 * BLOCK)
    dist = q_idx[:, None] + BLOCK - k_idx[None, :]
    in_window = (dist >= 0) & (dist < WINDOW)
    has_prev = (jnp.arange(nb)[:, None] > 0) | (k_idx[None, :] >= BLOCK)
    mask = in_window[None, :, :] & has_prev[:, None, :]
    bias = rel_bias[t5_bucket(dist)].astype(jnp.float32)
    bias = bias.transpose(2, 0, 1).reshape(ATTN_KV_HEADS, g, BLOCK, 2 * BLOCK)
    logits = jnp.where(mask[None, :, None, None, :, :], scores + bias, NEG_INF)
    sink = sinks.astype(jnp.float32).reshape(ATTN_KV_HEADS, g)[None, None, :, :, None, None]
    m = jnp.maximum(jnp.max(logits, axis=-1, keepdims=True), sink)
    p = jnp.exp(logits - m)
    probs = p / (jnp.sum(p, axis=-1, keepdims=True) + jnp.exp(sink - m))
    out = jnp.einsum('bnhgqk,bnkhd->bnqhgd', probs.astype(v.dtype), vband)
    return out.reshape(b, s, ATTN_WIDTH)


def rg_lru(x, w_a, b_a, w_x, b_x, lam):
    b, s, _ = x.shape
    xf = x.astype(jnp.float32)
    xb = xf.reshape(b, s, RG_HEADS, RG_HEAD_DIM)
    r = jax.nn.sigmoid(jnp.einsum('bshi,hij->bshj', xb, w_a.astype(jnp.float32)).reshape(b, s, RG_WIDTH)
                       + b_a.astype(jnp.float32))
    i = jax.nn.sigmoid(jnp.einsum('bshi,hij->bshj', xb, w_x.astype(jnp.float32)).reshape(b, s, RG_WIDTH)
                       + b_x.astype(jnp.float32))
    log_a = -RG_C * r * jax.nn.softplus(-lam.astype(jnp.float32))
    a = jnp.exp(log_a)
    u = jnp.sqrt(-jnp.expm1(2.0 * log_a)) * (i * xf)

    def combine(c1, c2):
        a1, b1 = c1
        a2, b2 = c2
        return a1 * a2, a2 * b1 + b2

    _, h = lax.associative_scan(combine, (a, u), axis=1)
    return h.astype(x.dtype)


def memory_cross_attention(h, mem_n, wq, wk, wv, wo):
    b, s, _ = h.shape
    q = (h @ wq).reshape(b, s, XA_HEADS, XA_HEAD_DIM)
    k = (mem_n @ wk).reshape(b, MEM_LEN, XA_HEADS, XA_HEAD_DIM)
    v = (mem_n @ wv).reshape(b, MEM_LEN, XA_HEADS, XA_HEAD_DIM)
    scores = jnp.einsum('bshd,bmhd->bhsm', q, k, preferred_element_type=jnp.float32) / math.sqrt(XA_HEAD_DIM)
    probs = jax.nn.softmax(scores, axis=-1).astype(v.dtype)
    out = jnp.einsum('bhsm,bmhd->bshd', probs, v).reshape(b, s, XA_WIDTH)
    return out @ wo


def swiglu(h, wg, wu, wd):
    return (jax.nn.silu(h @ wg) * (h @ wu)) @ wd


def moe_swiglu(h, router, wg, wu, wd):
    logits = (h @ router).astype(jnp.float32)
    top_val, top_idx = lax.top_k(logits, TOP_K)
    w = jax.nn.softmax(top_val, axis=-1)
    gates = jnp.sum(jax.nn.one_hot(top_idx, N_EXPERTS, dtype=jnp.float32) * w[..., None], axis=-2)
    out = jnp.zeros_like(h)
    for e in range(N_EXPERTS):
        out = out + gates[..., e:e + 1].astype(h.dtype) * swiglu(h, wg[e], wu[e], wd[e])
    return out


def setup_inputs(seed: int = 0) -> dict:
    key = jax.random.key(seed)
    ks = jax.random.split(key, 32)
    L = DEPTH

    def nrm(k, shape, scale):
        return jax.random.normal(k, shape, jnp.float32) * scale

    def gain(k, shape):
        return 1.0 + 0.05 * jax.random.normal(k, shape, jnp.float32)

    a0 = jax.random.uniform(ks[14], (L, RG_WIDTH), jnp.float32, minval=0.9, maxval=0.999)
    return {
        "x": nrm(ks[0], (BATCH, SEQ, D_MODEL), 1.0),
        "mem": nrm(ks[1], (BATCH, MEM_LEN, D_MODEL), 1.0),
        "rel_bias": nrm(ks[2], (N_BUCKETS, ATTN_Q_HEADS), 0.5),
        "mix_norm": gain(ks[3], (L, D_MODEL)),
        "w_in": nrm(ks[4], (L, D_MODEL, IN_COLS), D_MODEL ** -0.5),
        "attn_sinks": nrm(ks[5], (L, ATTN_Q_HEADS), 0.5),
        "sc_conv_w": nrm(ks[6], (L, SC_CONV, SC_WIDTH), SC_CONV ** -0.5),
        "sc_conv_b": nrm(ks[7], (L, SC_WIDTH), 0.01),
        "rg_conv_w": nrm(ks[8], (L, RG_CONV, RG_WIDTH), RG_CONV ** -0.5),
        "rg_conv_b": nrm(ks[9], (L, RG_WIDTH), 0.01),
        "rg_w_a": nrm(ks[10], (L, RG_HEADS, RG_HEAD_DIM, RG_HEAD_DIM), RG_HEAD_DIM ** -0.5),
        "rg_b_a": nrm(ks[11], (L, RG_WIDTH), 0.01),
        "rg_w_x": nrm(ks[12], (L, RG_HEADS, RG_HEAD_DIM, RG_HEAD_DIM), RG_HEAD_DIM ** -0.5),
        "rg_b_x": nrm(ks[13], (L, RG_WIDTH), 0.01),
        "rg_lambda": jnp.log(a0) - jnp.log1p(-a0),
        "w_out": nrm(ks[15], (L, D_MIX, D_MODEL), D_MIX ** -0.5),
        "xa_norm": gain(ks[16], (L, D_MODEL)),
        "mem_norm": gain(ks[17], (L, D_MODEL)),
        "xa_wq": nrm(ks[18], (L, D_MODEL, XA_WIDTH), D_MODEL ** -0.5),
        "xa_wk": nrm(ks[19], (L, D_MODEL, XA_WIDTH), D_MODEL ** -0.5),
        "xa_wv": nrm(ks[20], (L, D_MODEL, XA_WIDTH), D_MODEL ** -0.5),
        "xa_wo": nrm(ks[21], (L, XA_WIDTH, D_MODEL), XA_WIDTH ** -0.5),
        "ffn_norm": gain(ks[22], (L, D_MODEL)),
        "dense_wg": nrm(ks[23], (N_DENSE, D_MODEL, D_FF_DENSE), D_MODEL ** -0.5),
        "dense_wu": nrm(ks[24], (N_DENSE, D_MODEL, D_FF_DENSE), D_MODEL ** -0.5),
        "dense_wd": nrm(ks[25], (N_DENSE, D_FF_DENSE, D_MODEL), D_FF_DENSE ** -0.5),
        "moe_router": nrm(ks[26], (N_MOE, D_MODEL, N_EXPERTS), D_MODEL ** -0.5),
        "moe_wg": nrm(ks[27], (N_MOE, N_EXPERTS, D_MODEL, D_FF_EXPERT), D_MODEL ** -0.5),
        "moe_wu": nrm(ks[28], (N_MOE, N_EXPERTS, D_MODEL, D_FF_EXPERT), D_MODEL ** -0.5),
        "moe_wd": nrm(ks[29], (N_MOE, N_EXPERTS, D_FF_EXPERT, D_MODEL), D_FF_EXPERT ** -0.5),
        "final_norm": gain(ks[30], (D_MODEL,)),
    }


def reference(x, mem, rel_bias, mix_norm, w_in, attn_sinks, sc_conv_w, sc_conv_b,
              rg_conv_w, rg_conv_b, rg_w_a, rg_b_a, rg_w_x, rg_b_x, rg_lambda, w_out,
              xa_norm, mem_norm, xa_wq, xa_wk, xa_wv, xa_wo, ffn_norm,
              dense_wg, dense_wu, dense_wd, moe_router, moe_wg, moe_wu, moe_wd, final_norm):
    widths = [ATTN_WIDTH, KV_WIDTH, KV_WIDTH, SC_WIDTH, SC_WIDTH, SC_WIDTH, RG_WIDTH, RG_WIDTH]
    offsets = [int(o) for o in np.cumsum(widths)[:-1]]
    for layer in range(DEPTH):
        h = rms_norm(x, mix_norm[layer])
        proj = h @ w_in[layer]
        q, k, v, sc_b, sc_c, sc_x, rg_x, rg_g = jnp.split(proj, offsets, axis=-1)
        attn_out = sliding_window_sink_attention(q, k, v, attn_sinks[layer], rel_bias)
        conv_out = sc_b * causal_dwconv(sc_c * sc_x, sc_conv_w[layer], sc_conv_b[layer])
        rg_in = causal_dwconv(rg_x, rg_conv_w[layer], rg_conv_b[layer])
        rg_out = rg_lru(rg_in, rg_w_a[layer], rg_b_a[layer], rg_w_x[layer], rg_b_x[layer],
                        rg_lambda[layer]) * jax.nn.gelu(rg_g)
        mixed = jnp.concatenate([attn_out, conv_out, rg_out], axis=-1)
        x = x + mixed @ w_out[layer]
        hx = rms_norm(x, xa_norm[layer])
        mem_n = rms_norm(mem, mem_norm[layer])
        x = x + memory_cross_attention(hx, mem_n, xa_wq[layer], xa_wk[layer], xa_wv[layer], xa_wo[layer])
        hf = rms_norm(x, ffn_norm[layer])
        if layer % 2 == 0:
            j = layer // 2
            x = x + swiglu(hf, dense_wg[j], dense_wu[j], dense_wd[j])
        else:
            j = layer // 2
            x = x + moe_swiglu(hf, moe_router[j], moe_wg[j], moe_wu[j], moe_wd[j])
    return rms_norm(x, final_norm)
```

```python
import contextlib
import math
import numpy as np
import concourse.bass as bass
import concourse.mybir as mybir
from concourse.bass_utils import run_bass_kernel_spmd

F32 = mybir.dt.float32
BF16 = mybir.dt.bfloat16
ALU = mybir.AluOpType
AF = mybir.ActivationFunctionType
AX = mybir.AxisListType

D = 1024
KC = 8
T = 512
NSUB = 4
SEQ = 4096
NCORES = 8
INC = 2176
DFF = 2816
DFE = 3584
NE = 8
SLOT = 6144
NSLOT = 4
NEG = -1.0e30
SELF_SYNC = True
SELF_WAR = False
DEBUG = {"att": True, "sc": True, "rg": True, "wout": True}


class Prog:
    def __init__(self, nc, es):
        self.nc = nc
        self.es = es
        self.engs = {"pe": nc.tensor, "act": nc.scalar, "dve": nc.vector, "pool": nc.gpsimd, "sp": nc.sync}
        self.sems = {}
        self.cnt = {}
        for n in self.engs:
            self.sems[n] = es.enter_context(nc.semaphore("sem_" + n))
            self.cnt[n] = 0
        self.waited = {n: {} for n in self.engs}
        self.lastw = {}
        self.readers = {}

    def new_sem(self, key):
        if key not in self.sems:
            self.sems[key] = self.es.enter_context(self.nc.semaphore("sem_" + key.replace(":", "_")))
            self.cnt[key] = 0

    def _deps(self, reads, writes, extra=()):
        deps = {}

        def add(d):
            if d is not None:
                deps[d[0]] = max(deps.get(d[0], 0), d[1])

        for k in reads:
            add(self.lastw.get(k))
        for k in writes:
            add(self.lastw.get(k))
        self._raw_self = dict(deps)
        for k in writes:
            for sk, v in self.readers.get(k, {}).items():
                add((sk, v))
        for d in extra:
            add(d)
        return deps

    def _wait(self, x, deps):
        for sk, v in deps.items():
            if sk == x and (x == "pe" or not SELF_SYNC):
                continue
            if sk == x and not SELF_WAR:
                v = self._raw_self.get(x, 0)
                if v == 0:
                    continue
            if self.waited[x].get(sk, 0) >= v:
                continue
            self.engs[x].wait_ge(self.sems[sk], v)
            self.waited[x][sk] = v

    def op(self, x, fn, reads=(), writes=()):
        self._wait(x, self._deps(reads, writes))
        ins = fn(self.engs[x])
        self.cnt[x] += 1
        ins.then_inc(self.sems[x], 1)
        c = self.cnt[x]
        for k in reads:
            self.readers.setdefault(k, {})[x] = c
        for k in writes:
            self.lastw[k] = (x, c)
            self.readers[k] = {}

    def dma(self, q, pairs, semkey, reads=(), writes=(), extra=()):
        self.new_sem(semkey)
        self._wait(q, self._deps(reads, writes, extra))
        for out, in_ in pairs:
            ins = self.engs[q].dma_start(out=out, in_=in_)
            ins.then_inc(self.sems[semkey], 16)
            self.cnt[semkey] += 16
        c = self.cnt[semkey]
        for k in reads:
            self.readers.setdefault(k, {})[semkey] = c
        for k in writes:
            self.lastw[k] = (semkey, c)
            self.readers[k] = {}


def build_program(nc, NT, stop_after=99, nexp=NE):
    S = NT * T
    es = contextlib.ExitStack()
    with es:
        P = Prog(nc, es)

        def din(name, shape, dt=F32):
            return nc.dram_tensor(name, list(shape), dt, kind="ExternalInput").ap()

        def dint(name, shape, dt=BF16):
            return nc.dram_tensor(name, list(shape), dt, kind="Internal").ap()

        x_d = din("x", [S, D])
        mem_d = din("mem", [256, D])
        y_d = nc.dram_tensor("y", [S, D], F32, kind="ExternalOutput").ap()
        w = {}
        wshapes = {
            "w_inp": [2, D, INC], "w_out": [2, D, D], "xa_wq": [2, D, 512], "xa_wk": [2, D, 512],
            "xa_wv": [2, D, 512], "xa_wo": [2, 512, D], "dense_wg": [1, D, DFF], "dense_wu": [1, D, DFF],
            "dense_wd": [1, DFF, D], "moe_wg": [NE, D, DFE], "moe_wu": [NE, D, DFE], "moe_wd": [NE, DFE, D],
        }
        w16 = {}
        for k, shp in wshapes.items():
            w[k] = din(k, shp)
            w16[k] = dint(k + "_16", shp)
        router_d = din("router", [D, NE])
        gains_d = din("gains", [9, D])
        biasg_d = din("biasg", [128, 8, 256])
        maskc_d = din("maskc", [128, 256])
        sinks_d = din("sinks", [1, 16])
        chanp_d = din("chanp", [128, 48])
        rgw_d = din("rgw", [2, 2, 4, 64, 64])
        ident_d = din("ident", [128, 128])

        def sb(name, shape, dt=F32):
            return es.enter_context(nc.sbuf_tensor(name, list(shape), dt))

        xres = sb("xres", [128, NSUB, D])
        bias8 = sb("bias8", [128, 8, 256])
        kdup = sb("kdup", [128, 2, 2, 128 + T], BF16)
        vpad = sb("vpad", [128, 5, 2, 2, 128], BF16)
        kcar = sb("kcar", [128, 2, 4, 128], BF16)
        vcar = sb("vcar", [128, 2, 4, 128], BF16)
        ucar = sb("ucar", [128, 2, 2, 2])
        xcar = sb("xcar", [128, 2, 2, 3])
        hcar = sb("hcar", [128, 2, 2, 1])
        kmT = sb("kmT", [128, 2, 4, 256], BF16)
        vm = sb("vm", [128, 2, 2, 512], BF16)
        ident32 = sb("ident32", [128, 128])
        identb = sb("identb", [128, 128], BF16)
        chanp = sb("chanp_sb", [128, 48])
        dcp = sb("dcp", [128, 2, 2, 2])
        sinks = sb("sinks_sb", [128, 16])
        BD = sb("BD", [128, 2, 2, 2, 128], BF16)
        router_sb = sb("router_sb", [128, KC, NE])
        maskc = sb("maskc_sb", [128, 256])
        small = sb("small", [128, 128])
        xn = [sb("xn0", [128, D]), sb("xn1", [128, D])]
        gbc = [sb("gbc0", [128, D]), sb("gbc1", [128, D])]
        hT = sb("hT", [128, KC, T], BF16)
        hT32 = sb("hT32", [128, KC, 128])
        qT = sb("qT", [128, 4, T], BF16)
        NBR = 5
        br = [sb(f"br{i}", [128, T]) for i in range(NBR)]
        ubuf = sb("ubuf", [128, 2 + T])
        xbuf = sb("xbuf", [128, 3 + T])
        tA = sb("tA", [128, T])
        tB = sb("tB", [128, T])
        tC = sb("tC", [128, T])
        tD = sb("tD", [128, T])
        rg16 = sb("rg16", [128, T], BF16)
        Lb = sb("Lb", [128, 8, 256])
        Pn = sb("Pn", [128, 8, 256], BF16)
        PT = sb("PT", [128, 8, 2, 128], BF16)
        mixT = sb("mixT", [128, KC, T], BF16)
        h1 = [sb("h1a", [128, 2, T], BF16), sb("h1b", [128, 2, T], BF16)]
        sil = [sb("sil0", [128, T]), sb("sil1", [128, T])]
        gates = sb("gates", [128, NSUB, NE])
        slots = [sb(f"slot{i}", [128, SLOT], BF16) for i in range(NSLOT)]
        psb = [es.enter_context(nc.psum_tensor(f"ps{i}", [128, 512], F32)) for i in range(8)]

        _sc = [0]

        def sm(n, key):
            a = small[:, _sc[0]:_sc[0] + n]
            _sc[0] += n
            assert _sc[0] <= 128
            return a

        epsc = sm(1, "c")
        onec = sm(1, "c")
        ss = sm(1, "ss")
        rs = sm(1, "rs")
        rstd = sm(1, "rstd")
        mx = sm(8, "mx")
        negm = sm(8, "negm")
        rsum = sm(8, "rsum")
        dsk = sm(8, "dsk")
        rec = sm(8, "rec")
        ss4 = sm(4, "ss4")
        rs4 = sm(4, "rs4")
        rstd4 = sm(4, "rstd4")
        lg = sm(8, "lg")
        top8 = sm(8, "top8")
        nm1 = sm(1, "nm1")
        sel = sm(8, "sel")
        ex = sm(8, "ex")
        gsm = sm(1, "gsm")
        grc = sm(1, "grc")
        lam_t = sm(8, "lam")

        rot = {"mm": [0, 1, 2, 3], "tr": [4, 5], "att": [6, 7], "dn": [4, 5, 6, 7], "att4": [4, 5, 6, 7]}
        rpos = {"mm": 0, "tr": 0, "att": 0, "dn": 0, "att4": 0}

        def bank(role):
            i = rot[role][rpos[role] % len(rot[role])]
            rpos[role] += 1
            return i

        def pk(i):
            return f"ps{i}"

        P.new_sem("const")
        cpairs = [
            (ident32[:], ident_d), (chanp[:], chanp_d), (maskc[:], maskc_d), (bias8[:], biasg_d),
            (sinks[:], sinks_d[0, :].partition_broadcast(128)),
            (router_sb[:], router_d.rearrange("(k p) e -> p k e", p=128)),
        ]
        ckeys = ["ident32", "chanp", "maskc", "bias8", "sinks", "router"]
        P.dma("sp", cpairs, "const", writes=ckeys)

        def pset(ap, val, key):
            P.op("pool", lambda e: e.memset(ap, val), writes=[key])

        pset(BD[:], 0.0, "BD")
        pset(vpad[:], 0.0, "vpad_all")
        pset(kdup[:], 0.0, "kdup")
        pset(kcar[:], 0.0, "kcar")
        pset(vcar[:], 0.0, "vcar")
        pset(ucar[:], 0.0, "ucar")
        pset(xcar[:], 0.0, "xcar")
        pset(hcar[:], 0.0, "hcar")
        pset(epsc, 1e-6, "c")
        pset(onec, 1.0, "c")
        bdpairs = []
        for l in range(2):
            for ax in range(2):
                for hh in range(4):
                    i, o = hh // 2, (hh % 2) * 64
                    bdpairs.append((BD[o:o + 64, l, ax, i, o:o + 64], rgw_d[l, ax, hh]))
        P.dma("pool", bdpairs, "bdld", writes=["BD"])

        cast_total = {}

        cast_tot = {}
        cast_pending = []
        cast_issued = []

        def cast_issue(item):
            grp, d_, s_ = item
            nc.gpsimd.dma_start(out=d_, in_=s_).then_inc(P.sems["cast:" + grp], 16)
            P.cnt["cast:" + grp] += 16
            cast_issued.append(("cast:" + grp, P.cnt["cast:" + grp]))

        def cast_tick(n=1):
            for _ in range(n):
                if not cast_pending:
                    return
                nc.gpsimd.wait_ge(P.sems["pe"], P.cnt["pe"])
                if len(cast_issued) >= 2:
                    sk, v = cast_issued[-2]
                    nc.gpsimd.wait_ge(P.sems[sk], v)
                cast_issue(cast_pending.pop(0))

        def cast2d(src, dst, grp, defer=False):
            P.new_sem("cast:" + grp)
            R, C = src.shape
            ns = (C + 2047) // 2048
            assert C % ns == 0
            RB = 1024 if C <= 2048 else 512
            for r0 in range(0, R, RB):
                r1 = min(R, r0 + RB)
                s_ = src[r0:r1, :].rearrange("r (a c) -> r a c", a=ns)
                d_ = dst[r0:r1, :].rearrange("r (a c) -> r a c", a=ns)
                cast_tot[grp] = cast_tot.get(grp, 0) + 16
                if defer:
                    cast_pending.append((grp, d_, s_))
                else:
                    cast_issue((grp, d_, s_))

        def cast_fence(grp):
            nc.gpsimd.wait_ge(P.sems["cast:" + grp], P.cnt["cast:" + grp])

        for l in range(2):
            cast2d(w["xa_wk"][l], w16["xa_wk"][l], "A")
            cast2d(w["xa_wv"][l], w16["xa_wv"][l], "A")
        for k in ["w_inp", "w_out", "xa_wq", "xa_wo"]:
            cast2d(w[k][0], w16[k][0], "A")
        cast_fence("A")
        for k in ["dense_wg", "dense_wu", "dense_wd"]:
            cast2d(w[k][0], w16[k][0], "B")
        for k in ["w_inp", "w_out", "xa_wq", "xa_wo"]:
            cast2d(w[k][1], w16[k][1], "C")
        cast_fence("B")
        for e in range(nexp):
            P.new_sem(f"cast:E{e}")

        def cast_experts():
            cast_tick(len([1 for it in cast_pending if it[0] == "E0"]))

        for e in range(nexp):
            for k in ["moe_wg", "moe_wu", "moe_wd"]:
                cast2d(w[k][e], w16[k][e], f"E{e}", defer=True)

        def castdep(grp):
            return ("cast:" + grp, cast_tot[grp])

        def kview(slot, off, k, n):
            return slot[:, off:off + k * n].rearrange("p (k n) -> p k n", k=k)

        def piece_cols(name, src2d, c0, c1, grp, k=KC):
            n = c1 - c0
            return dict(name=name, grp=grp,
                        parts=[(lambda s, k=k, n=n: kview(s, 0, k, n),
                                src2d[:, c0:c1].rearrange("(k p) n -> p k n", p=128))])

        def piece_ffn(name, wg, wu, wd, gi, grp):
            f0 = gi * 256
            return dict(name=name, grp=grp, parts=[
                (lambda s: kview(s, 0, KC, 256), wg[:, f0:f0 + 256].rearrange("(k p) n -> p k n", p=128)),
                (lambda s: kview(s, 2048, KC, 256), wu[:, f0:f0 + 256].rearrange("(k p) n -> p k n", p=128)),
                (lambda s: kview(s, 4096, 2, D), wd[f0:f0 + 256, :].rearrange("(c p) n -> p c n", p=128)),
            ])

        sched = []
        for l in range(2):
            sched.append(piece_cols(f"xa_wk{l}", w16["xa_wk"][l], 0, 512, "A"))
            sched.append(piece_cols(f"xa_wv{l}", w16["xa_wv"][l], 0, 512, "A"))
        NSTAGE = 0
        for t in range(NT):
            for l in range(2):
                g = "A" if l == 0 else "C"
                st = l * 3
                if st < stop_after:
                    for pc in range(5):
                        c0, c1 = pc * 512, min(INC, pc * 512 + 512)
                        sched.append(piece_cols(f"w_in{l}_{pc}", w16["w_inp"][l], c0, c1, g))
                    for hf in range(2):
                        sched.append(piece_cols(f"w_out{l}_{hf}", w16["w_out"][l], hf * 512, hf * 512 + 512, g))
                if st + 1 < stop_after:
                    sched.append(piece_cols(f"xa_wq{l}", w16["xa_wq"][l], 0, 512, g))
                    sched.append(piece_cols(f"xa_wo{l}", w16["xa_wo"][l], 0, D, g, k=4))
                if st + 2 < stop_after:
                    if l == 0:
                        for gi in range(DFF // 256):
                            sched.append(piece_ffn(f"dense_{gi}", w16["dense_wg"][0], w16["dense_wu"][0],
                                                   w16["dense_wd"][0], gi, "B"))
                    else:
                        for e in range(nexp):
                            for gi in range(DFE // 256):
                                sched.append(piece_ffn(f"moe{e}_{gi}", w16["moe_wg"][e], w16["moe_wu"][e],
                                                       w16["moe_wd"][e], gi, f"E{e}"))
        wstate = dict(issued=0, next=0)

        def acquire(name):
            i = wstate["next"]
            assert sched[i]["name"] == name, (sched[i]["name"], name)
            while wstate["issued"] < min(len(sched), i + NSLOT - 1):
                j = wstate["issued"]
                pcs = sched[j]
                sl = slots[j % NSLOT]
                P.dma("sp", [(mk(sl), src) for mk, src in pcs["parts"]], f"slot{j % NSLOT}",
                      writes=[f"slot{j % NSLOT}"], extra=[castdep(pcs["grp"])])
                wstate["issued"] += 1
            wstate["next"] += 1
            sl = slots[i % NSLOT]
            return [mk(sl) for mk, _ in sched[i]["parts"]], f"slot{i % NSLOT}"

        for l in range(2):
            for i in range(2):
                col = 24 * l + 18 + 3 * i + 2
                j = (l * 2 + i) * 2
                P.op("act", lambda e, col=col, j=j: e.activation(out=lam_t[:, j:j + 1], in_=chanp[:, col:col + 1],
                                                                 func=AF.Exp, scale=-1.0),
                     reads=["chanp"], writes=["lam"])
                P.op("act", lambda e, j=j: e.activation(out=lam_t[:, j + 1:j + 2], in_=lam_t[:, j:j + 1],
                                                        func=AF.Ln, bias=onec, scale=1.0),
                     reads=["lam", "c"], writes=["lam"])
                P.op("dve", lambda e, l=l, i=i, j=j: e.tensor_scalar(out=dcp[:, l, i, 0:1], in0=lam_t[:, j + 1:j + 2],
                                                                     scalar1=-8.0, scalar2=None, op0=ALU.mult),
                     reads=["lam"], writes=["dcp"])
                P.op("dve", lambda e, l=l, i=i, j=j: e.tensor_scalar(out=dcp[:, l, i, 1:2], in0=lam_t[:, j + 1:j + 2],
                                                                     scalar1=-16.0, scalar2=None, op0=ALU.mult),
                     reads=["lam"], writes=["dcp"])
        for h in range(8):
            P.op("dve", lambda e, h=h: e.tensor_tensor(out=bias8[:, h, :], in0=bias8[:, h, :], in1=maskc[:], op=ALU.add),
                 reads=["bias8", "maskc"], writes=["bias8"])
        P.op("dve", lambda e: e.tensor_copy(out=identb[:], in_=ident32[:]), reads=["ident32"], writes=["identb"])

        gstate = dict(g=0, n=0)

        def load_gain(row):
            j = gstate["g"] % 2
            gstate["g"] += 1
            P.dma("sp", [(gbc[j][:], gains_d[row, :].partition_broadcast(128))], f"gbc{j}", writes=[f"gbc{j}"])
            return j

        def evac(eng, out, in_, reads, writes):
            if eng == "act":
                P.op("act", lambda e: e.activation(out=out, in_=in_, func=AF.Copy), reads=reads, writes=writes)
            else:
                P.op("dve", lambda e: e.tensor_copy(out=out, in_=in_), reads=reads, writes=writes)

        def norm(srcs, row, mode, col0=0, router=False, ybase=None):
            gj = load_gain(row)
            ns_ = len(srcs)
            junk = Lb[:].rearrange("p h k -> p (h k)")
            for si, (xa, xkey) in enumerate(srcs):
                xkeys = list(xkey) if isinstance(xkey, (list, tuple)) else [xkey]
                P.op("act", lambda e: e.activation(out=junk[:, (si % 2) * D:(si % 2 + 1) * D], in_=xa, func=AF.Square,
                                                   accum_out=ss4[:, si:si + 1]),
                     reads=xkeys, writes=["Lb", "ss4"])
            P.op("act", lambda e: e.activation(out=rs4[:, 0:ns_], in_=ss4[:, 0:ns_], func=AF.Sqrt, bias=epsc, scale=1.0 / D),
                 reads=["ss4", "c"], writes=["rs4"])
            P.op("dve", lambda e: e.reciprocal(out=rstd4[:, 0:ns_], in_=rs4[:, 0:ns_]), reads=["rs4"], writes=["rstd4"])
            for si, (xa, xkey) in enumerate(srcs):
                j = gstate["n"] % 2
                gstate["n"] += 1
                xb = xn[j]
                xk = f"xn{j}"
                xkeys = list(xkey) if isinstance(xkey, (list, tuple)) else [xkey]
                P.op("dve", lambda e: e.scalar_tensor_tensor(out=xb[:], in0=xa, scalar=rstd4[:, si:si + 1], in1=gbc[gj][:],
                                                             op0=ALU.mult, op1=ALU.mult),
                     reads=xkeys + ["rstd4", f"gbc{gj}"], writes=[xk])
                if mode == "store":
                    P.dma("act", [(y_d[ybase + si * 128: ybase + (si + 1) * 128, :], xb[:])], f"ost{j}", reads=[xk])
                    continue
                b0, b1 = bank("tr"), bank("tr")
                for kc in range(KC):
                    bi = b0 if kc < 4 else b1
                    P.op("pe", lambda e, kc=kc, bi=bi: e.transpose(out=psb[bi][:, (kc % 4) * 128:(kc % 4 + 1) * 128],
                                                                  in_=xb[:, kc * 128:(kc + 1) * 128], identity=ident32[:]),
                         reads=[xk, "ident32"], writes=[pk(bi)])
                c = col0 + si * 128
                evac("act", hT[:, 0:4, c:c + 128], psb[b0][:].rearrange("p (k n) -> p k n", k=4), [pk(b0)], ["hT"])
                evac("dve", hT[:, 4:8, c:c + 128], psb[b1][:].rearrange("p (k n) -> p k n", k=4), [pk(b1)], ["hT"])
                if router:
                    evac("act", hT32[:, 0:4, :], psb[b0][:].rearrange("p (k n) -> p k n", k=4), [pk(b0)], ["hT32"])
                    evac("dve", hT32[:, 4:8, :], psb[b1][:].rearrange("p (k n) -> p k n", k=4), [pk(b1)], ["hT32"])
                    bl = bank("mm")
                    for kc in range(KC):
                        P.op("pe", lambda e, kc=kc: e.matmul(psb[bl][:, 0:NE], lhsT=hT32[:, kc, :], rhs=router_sb[:, kc, :],
                                                             start=(kc == 0), stop=(kc == KC - 1)),
                             reads=["hT32", "router"], writes=[pk(bl)])
                    P.op("dve", lambda e: e.tensor_copy(out=lg, in_=psb[bl][:, 0:NE]), reads=[pk(bl)], writes=["lg"])
                    P.op("dve", lambda e: e.max(out=top8, in_=lg), reads=["lg"], writes=["top8"])
                    P.op("dve", lambda e: e.tensor_scalar(out=nm1, in0=top8[:, 0:1], scalar1=-1.0, scalar2=None,
                                                          op0=ALU.mult), reads=["top8"], writes=["nm1"])
                    P.op("dve", lambda e: e.tensor_scalar(out=sel, in0=lg, scalar1=top8[:, 1:2], scalar2=None,
                                                          op0=ALU.is_ge), reads=["lg", "top8"], writes=["sel"])
                    P.op("act", lambda e: e.activation(out=ex, in_=lg, func=AF.Exp, bias=nm1, scale=1.0),
                         reads=["lg", "nm1"], writes=["ex"])
                    P.op("dve", lambda e: e.tensor_tensor(out=ex, in0=ex, in1=sel, op=ALU.mult),
                         reads=["ex", "sel"], writes=["ex"])
                    P.op("dve", lambda e: e.tensor_reduce(out=gsm, in_=ex, axis=AX.X, op=ALU.add),
                         reads=["ex"], writes=["gsm"])
                    P.op("dve", lambda e: e.reciprocal(out=grc, in_=gsm), reads=["gsm"], writes=["grc"])
                    P.op("dve", lambda e, si=si: e.tensor_scalar(out=gates[:, si, :], in0=ex, scalar1=grc, scalar2=None,
                                                                 op0=ALU.mult), reads=["ex", "grc"], writes=["gates"])

        def proj_fm(wv_, wkey, cidx, n_tok, evac_eng, out_ap, out_key, kcn=KC, rhs_cols=None):
            bi = bank("mm")
            c0, c1 = rhs_cols if rhs_cols else (0, n_tok)
            for kc in range(kcn):
                P.op("pe", lambda e, kc=kc: e.matmul(psb[bi][:, 0:n_tok], lhsT=wv_[:, kc, cidx * 128:(cidx + 1) * 128],
                                                     rhs=hT[:, kc, c0:c1], start=(kc == 0), stop=(kc == kcn - 1)),
                     reads=[wkey, "hT"], writes=[pk(bi)])
            evac(evac_eng, out_ap, psb[bi][:, 0:n_tok], [pk(bi)], [out_key])

        def softmax_block(nh, sink_ap):
            P.op("dve", lambda e: e.tensor_reduce(out=mx[:, 0:nh], in_=Lb[:, 0:nh, :], axis=AX.X, op=ALU.max),
                 reads=["Lb"], writes=["mx"])
            if sink_ap is not None:
                P.op("dve", lambda e: e.tensor_tensor(out=mx[:, 0:nh], in0=mx[:, 0:nh], in1=sink_ap, op=ALU.max),
                     reads=["mx", "sinks"], writes=["mx"])
            P.op("dve", lambda e: e.tensor_scalar(out=negm[:, 0:nh], in0=mx[:, 0:nh], scalar1=-1.0, scalar2=None,
                                                  op0=ALU.mult), reads=["mx"], writes=["negm"])
            for hh in range(nh):
                P.op("act", lambda e, hh=hh: e.activation(out=Lb[:, hh, :], in_=Lb[:, hh, :], func=AF.Exp,
                                                          bias=negm[:, hh:hh + 1], scale=1.0,
                                                          accum_out=rsum[:, hh:hh + 1]),
                     reads=["Lb", "negm"], writes=["Lb", "rsum"])
            if sink_ap is not None:
                P.op("dve", lambda e: e.tensor_tensor(out=dsk[:, 0:nh], in0=sink_ap, in1=mx[:, 0:nh], op=ALU.subtract),
                     reads=["mx", "sinks"], writes=["dsk"])
                P.op("act", lambda e: e.activation(out=dsk[:, 0:nh], in_=dsk[:, 0:nh], func=AF.Exp),
                     reads=["dsk"], writes=["dsk"])
                P.op("dve", lambda e: e.tensor_tensor(out=rsum[:, 0:nh], in0=rsum[:, 0:nh], in1=dsk[:, 0:nh], op=ALU.add),
                     reads=["rsum", "dsk"], writes=["rsum"])
            P.op("dve", lambda e: e.reciprocal(out=rec[:, 0:nh], in_=rsum[:, 0:nh]), reads=["rsum"], writes=["rec"])
            for hh in range(nh):
                if hh % 2 == 0:
                    P.op("dve", lambda e, hh=hh: e.tensor_scalar(out=Pn[:, hh, :], in0=Lb[:, hh, :],
                                                                 scalar1=rec[:, hh:hh + 1], scalar2=None, op0=ALU.mult),
                         reads=["Lb", "rec"], writes=[f"Pn{hh}"])
                else:
                    P.op("act", lambda e, hh=hh: e.activation(out=Pn[:, hh, :], in_=Lb[:, hh, :], func=AF.Copy,
                                                              scale=rec[:, hh:hh + 1]),
                         reads=["Lb", "rec"], writes=[f"Pn{hh}"])
            nb_ = (nh + 3) // 4
            bts = [bank("mm") for _ in range(nb_)]
            for hh in range(nh):
                bt = bts[hh // 4]
                pv = psb[bt][:].bitcast(BF16)
                for kb in range(2):
                    o = ((hh % 4) * 2 + kb) * 128
                    P.op("pe", lambda e, hh=hh, kb=kb, o=o, pv=pv: e.transpose(out=pv[:, o:o + 128],
                                                                              in_=Pn[:, hh, kb * 128:(kb + 1) * 128],
                                                                              identity=identb[:]),
                         reads=[f"Pn{hh}", "identb"], writes=[pk(bt)])
            ptf = PT[:].rearrange("p h k n -> p (h k n)")
            for g_ in range(nb_):
                n_ = min(4, nh - 4 * g_) * 256
                evac("act" if g_ == 0 else "dve", ptf[:, g_ * 1024:g_ * 1024 + n_],
                     psb[bts[g_]][:].bitcast(BF16)[:, 0:n_], [pk(bts[g_])], ["PT"])

        P.dma("sp", [(xres[:, 0:2, :], mem_d.rearrange("(s p) d -> p s d", p=128))], "xld", writes=["x0", "x0b", "x1", "x1b"])
        for l in range(2):
            norm([(xres[:, 0, :], ["x0", "x0b"]), (xres[:, 1, :], ["x1", "x1b"])], 7 + l, "T")
            (wk_,), wkey = acquire(f"xa_wk{l}")
            for h in range(4):
                proj_fm(wk_, wkey, h, 256, "act" if h % 2 == 0 else "dve", kmT[:, l, h, :], "kmT")
            (wv_,), wkey = acquire(f"xa_wv{l}")
            for mt in range(2):
                bi = bank("mm")
                for kc in range(KC):
                    P.op("pe", lambda e, kc=kc: e.matmul(psb[bi][:], lhsT=hT[:, kc, mt * 128:(mt + 1) * 128],
                                                         rhs=wv_[:, kc, :], start=(kc == 0), stop=(kc == KC - 1)),
                         reads=[wkey, "hT"], writes=[pk(bi)])
                evac("act", vm[:, l, mt, :], psb[bi][:], [pk(bi)], ["vm"])

        xk_ = [f"x{s}" for s in range(NSUB)]
        xkh = lambda s: [f"x{s}", f"x{s}b"]
        xall = [k for s in range(NSUB) for k in xkh(s)]

        def resid_add(s, hf, bi, gate_ap=None):
            xa = xres[:, s, hf * 512:(hf + 1) * 512]
            if gate_ap is None:
                P.op("dve", lambda e: e.tensor_tensor(out=xa, in0=psb[bi][:], in1=xa, op=ALU.add),
                     reads=[pk(bi), xkh(s)[hf]], writes=[xkh(s)[hf]])
            else:
                P.op("dve", lambda e: e.scalar_tensor_tensor(out=xa, in0=psb[bi][:], scalar=gate_ap, in1=xa,
                                                             op0=ALU.mult, op1=ALU.add),
                     reads=[pk(bi), xkh(s)[hf], "gates"], writes=[xkh(s)[hf]])

        def sc_branch(l, i, kB, kC_, kX, B, C, X):
            cb = 24 * l + i * 4
            P.op("dve", lambda e: e.tensor_copy(out=ubuf[:, 0:2], in_=ucar[:, l, i, :]), reads=["ucar"], writes=["ubuf"])
            P.op("dve", lambda e: e.tensor_tensor(out=ubuf[:, 2:2 + T], in0=C, in1=X, op=ALU.mult),
                 reads=[kC_, kX], writes=["ubuf"])
            P.op("dve", lambda e: e.tensor_copy(out=ucar[:, l, i, :], in_=ubuf[:, T:T + 2]), reads=["ubuf"], writes=["ucar"])
            P.op("dve", lambda e: e.tensor_scalar(out=tA[:], in0=ubuf[:, 0:T], scalar1=chanp[:, cb:cb + 1],
                                                  scalar2=chanp[:, cb + 3:cb + 4], op0=ALU.mult, op1=ALU.add),
                 reads=["ubuf", "chanp"], writes=["tA"])
            for k in (1, 2):
                P.op("dve", lambda e, k=k: e.scalar_tensor_tensor(out=tA[:], in0=ubuf[:, k:k + T],
                                                                  scalar=chanp[:, cb + k:cb + k + 1], in1=tA[:],
                                                                  op0=ALU.mult, op1=ALU.add),
                     reads=["ubuf", "chanp", "tA"], writes=["tA"])
            P.op("dve", lambda e: e.tensor_tensor(out=mixT[:, 4 + i, :], in0=B, in1=tA[:], op=ALU.mult),
                 reads=[kB, "tA"], writes=["mixT"])

        def rg_branch(l, i, kR, kG, R, G):
            cb = 24 * l + 8 + i * 5
            cg = 24 * l + 18 + i * 3
            P.op("dve", lambda e: e.tensor_copy(out=xbuf[:, 0:3], in_=xcar[:, l, i, :]), reads=["xcar"], writes=["xbuf"])
            P.op("dve", lambda e: e.tensor_copy(out=xbuf[:, 3:3 + T], in_=R), reads=[kR], writes=["xbuf"])
            P.op("dve", lambda e: e.tensor_copy(out=xcar[:, l, i, :], in_=xbuf[:, T:T + 3]), reads=["xbuf"], writes=["xcar"])
            P.op("dve", lambda e: e.tensor_scalar(out=tA[:], in0=xbuf[:, 0:T], scalar1=chanp[:, cb:cb + 1],
                                                  scalar2=chanp[:, cb + 4:cb + 5], op0=ALU.mult, op1=ALU.add),
                 reads=["xbuf", "chanp"], writes=["tA"])
            for k in (1, 2, 3):
                P.op("dve", lambda e, k=k: e.scalar_tensor_tensor(out=tA[:], in0=xbuf[:, k:k + T],
                                                                  scalar=chanp[:, cb + k:cb + k + 1], in1=tA[:],
                                                                  op0=ALU.mult, op1=ALU.add),
                     reads=["xbuf", "chanp", "tA"], writes=["tA"])
            P.op("act", lambda e: e.activation(out=rg16[:], in_=tA[:], func=AF.Copy), reads=["tA"], writes=["rg16"])
            b_r, b_i = bank("mm"), bank("mm")
            P.op("pe", lambda e: e.matmul(psb[b_r][:], lhsT=BD[:, l, 0, i, :], rhs=rg16[:], start=True, stop=True),
                 reads=["BD", "rg16"], writes=[pk(b_r)])
            P.op("pe", lambda e: e.matmul(psb[b_i][:], lhsT=BD[:, l, 1, i, :], rhs=rg16[:], start=True, stop=True),
                 reads=["BD", "rg16"], writes=[pk(b_i)])
            P.op("act", lambda e: e.activation(out=tB[:], in_=psb[b_r][:], func=AF.Sigmoid, bias=chanp[:, cg:cg + 1], scale=1.0),
                 reads=[pk(b_r), "chanp"], writes=["tB"])
            P.op("act", lambda e: e.activation(out=tC[:], in_=psb[b_i][:], func=AF.Sigmoid, bias=chanp[:, cg + 1:cg + 2], scale=1.0),
                 reads=[pk(b_i), "chanp"], writes=["tC"])
            P.op("act", lambda e: e.activation(out=tD[:], in_=tB[:], func=AF.Exp, scale=dcp[:, l, i, 0:1]),
                 reads=["tB", "dcp"], writes=["tD"])
            P.op("act", lambda e: e.activation(out=tB[:], in_=tB[:], func=AF.Exp, scale=dcp[:, l, i, 1:2]),
                 reads=["tB", "dcp"], writes=["tB"])
            P.op("dve", lambda e: e.tensor_scalar(out=tB[:], in0=tB[:], scalar1=1.0, scalar2=-1.0, op0=ALU.min, op1=ALU.mult),
                 reads=["tB"], writes=["tB"])
            P.op("act", lambda e: e.activation(out=tB[:], in_=tB[:], func=AF.Sqrt, bias=onec, scale=1.0),
                 reads=["tB", "c"], writes=["tB"])
            P.op("dve", lambda e: e.tensor_tensor(out=tC[:], in0=tC[:], in1=tA[:], op=ALU.mult),
                 reads=["tC", "tA"], writes=["tC"])
            P.op("dve", lambda e: e.tensor_tensor(out=tC[:], in0=tC[:], in1=tB[:], op=ALU.mult),
                 reads=["tC", "tB"], writes=["tC"])
            P.op("dve", lambda e: e.tensor_tensor_scan(out=tA[:], data0=tD[:], data1=tC[:], initial=hcar[:, l, i, :],
                                                       op0=ALU.mult, op1=ALU.add),
                 reads=["tD", "tC", "hcar", "tA"], writes=["tA"])
            P.op("dve", lambda e: e.tensor_copy(out=hcar[:, l, i, :], in_=tA[:, T - 1:T]), reads=["tA"], writes=["hcar"])
            P.op("dve", lambda e: e.tensor_tensor(out=tB[:], in0=G, in1=G, op=ALU.mult), reads=[kG], writes=["tB"])
            P.op("dve", lambda e: e.tensor_scalar(out=tB[:], in0=tB[:], scalar1=0.044715, scalar2=1.0, op0=ALU.mult, op1=ALU.add),
                 reads=["tB"], writes=["tB"])
            P.op("dve", lambda e: e.tensor_tensor(out=tB[:], in0=tB[:], in1=G, op=ALU.mult), reads=["tB", kG], writes=["tB"])
            P.op("act", lambda e: e.activation(out=tB[:], in_=tB[:], func=AF.Sigmoid, scale=1.5957691216057308),
                 reads=["tB"], writes=["tB"])
            P.op("dve", lambda e: e.tensor_tensor(out=tB[:], in0=tB[:], in1=G, op=ALU.mult), reads=["tB", kG], writes=["tB"])
            P.op("dve", lambda e: e.tensor_tensor(out=mixT[:, 6 + i, :], in0=tA[:], in1=tB[:], op=ALU.mult),
                 reads=["tA", "tB"], writes=["mixT"])

        def mixer(l, t):
            norm([(xres[:, s, :], xkh(s)) for s in range(NSUB)], 3 * l + 0, "T")
            P.op("dve", lambda e: e.tensor_copy(out=kdup[:, :, :, 0:128], in_=kcar[:, l, :, :].rearrange("p (a b) n -> p a b n", a=2)), reads=["kcar"], writes=["kdup"])
            P.op("dve", lambda e: e.tensor_copy(out=vpad[:, 0, :, :, :], in_=vcar[:, l, :, :].rearrange("p (a b) n -> p a b n", a=2)),
                 reads=["vcar", "vpad_all"], writes=["vp0"])
            brpos = [0]
            brmap = {}

            def brnext(name):
                j = brpos[0] % NBR
                brpos[0] += 1
                brmap[name] = j
                return br[j][:], f"br{j}"

            for pc in range(5):
                (wv_,), wkey = acquire(f"w_in{l}_{pc}")
                if pc == 0:
                    for c in range(4):
                        proj_fm(wv_, wkey, c, T, "act" if c % 2 == 0 else "dve", qT[:, c, :], "qT")
                elif pc == 4:
                    for b in range(NSUB):
                        bi = bank("mm")
                        for kc in range(KC):
                            P.op("pe", lambda e, kc=kc: e.matmul(psb[bi][:, 0:128], lhsT=hT[:, kc, b * 128:(b + 1) * 128],
                                                                 rhs=wv_[:, kc, 0:128], start=(kc == 0), stop=(kc == KC - 1)),
                                 reads=[wkey, "hT"], writes=[pk(bi)])
                        for j in range(2):
                            for eo in range(2):
                                evac("act" if eo == 0 else "dve", vpad[:, b + 1, j, eo, eo * 64:eo * 64 + 64],
                                     psb[bi][:, j * 64:(j + 1) * 64], [pk(bi), "vpad_all"], [f"vp{b + 1}"])
                else:
                    names = {1: ["K0", "K1", "B0", "C0"], 2: ["X0", "B1", "C1", "X1"], 3: ["R0", "G0", "R1", "G1"]}[pc]
                    for c, nm in enumerate(names):
                        if nm[0] == "K":
                            j = int(nm[1])
                            bi = bank("mm")
                            for kc in range(KC):
                                P.op("pe", lambda e, kc=kc: e.matmul(psb[bi][:], lhsT=wv_[:, kc, c * 128:(c + 1) * 128],
                                                                     rhs=hT[:, kc, :], start=(kc == 0), stop=(kc == KC - 1)),
                                     reads=[wkey, "hT"], writes=[pk(bi)])
                            evac("act", kdup[0:64, j, 0, 128:128 + T], psb[bi][0:64, :], [pk(bi)], ["kdup"])
                            evac("dve", kdup[64:128, j, 1, 128:128 + T], psb[bi][64:128, :], [pk(bi)], ["kdup"])
                            continue
                        ap_, key_ = brnext(nm)
                        proj_fm(wv_, wkey, c, T, "act" if c % 2 == 0 else "dve", ap_, key_)
                        if nm in ("X0", "X1") and DEBUG["sc"]:
                            i = int(nm[1])
                            jb, jc, jx = brmap[f"B{i}"], brmap[f"C{i}"], brmap[f"X{i}"]
                            sc_branch(l, i, f"br{jb}", f"br{jc}", f"br{jx}", br[jb][:], br[jc][:], br[jx][:])
                        if nm in ("G0", "G1") and DEBUG["rg"]:
                            i = int(nm[1])
                            jr, jg = brmap[f"R{i}"], brmap[f"G{i}"]
                            rg_branch(l, i, f"br{jr}", f"br{jg}", br[jr][:], br[jg][:])
            for b in range(NSUB if DEBUG["att"] else 0):
                gb = t * NSUB + b
                ba = [bank("att4") for _ in range(4)]
                for h in range(8):
                    c, eo, hg = h // 2, h % 2, h // 4
                    bi = ba[h // 2]
                    P.op("pe", lambda e, c=c, eo=eo, bi=bi, h=h, hg=hg: e.matmul(
                        psb[bi][:, (h % 2) * 256:(h % 2 + 1) * 256],
                        lhsT=qT[:, c, b * 128:(b + 1) * 128],
                        rhs=kdup[:, hg, eo, b * 128:b * 128 + 256], start=True, stop=True),
                         reads=["qT", "kdup"], writes=[pk(bi)])
                for pr in range(4):
                    P.op("dve", lambda e, pr=pr: e.scalar_tensor_tensor(
                        out=Lb[:, 2 * pr:2 * pr + 2, :], in0=psb[ba[pr]][:].rearrange("p (h k) -> p h k", h=2),
                        scalar=0.125, in1=bias8[:, 2 * pr:2 * pr + 2, :], op0=ALU.mult, op1=ALU.add),
                         reads=[pk(ba[pr]), "bias8"], writes=["Lb"])
                if gb == 0:
                    P.op("dve", lambda e: e.memset(Lb[:, :, 0:128], NEG), writes=["Lb"])
                softmax_block(8, sinks[:, 8 * l:8 * l + 8])
                bo = bank("mm")
                for c in range(4):
                    for eo in range(2):
                        h = 2 * c + eo
                        for kb in range(2):
                            P.op("pe", lambda e, c=c, eo=eo, h=h, kb=kb: e.matmul(
                                psb[bo][:, c * 128:(c + 1) * 128], lhsT=vpad[:, b + kb, c // 2, eo, :],
                                rhs=PT[:, h, kb, :], start=(eo == 0 and kb == 0), stop=(eo == 1 and kb == 1)),
                                 reads=["PT", f"vp{b + kb}", "vpad_all"], writes=[pk(bo)])
                evac("dve", mixT[:, 0:4, b * 128:(b + 1) * 128],
                     psb[bo][:].rearrange("p (c n) -> p c n", c=4), [pk(bo)], ["mixT"])
            P.op("dve", lambda e: e.tensor_copy(out=kcar[:, l, :, :].rearrange("p (a b) n -> p a b n", a=2), in_=kdup[:, :, :, T:T + 128]), reads=["kdup"], writes=["kcar"])
            P.op("dve", lambda e: e.tensor_copy(out=vcar[:, l, :, :].rearrange("p (a b) n -> p a b n", a=2), in_=vpad[:, 4, :, :, :]),
                 reads=["vp4"], writes=["vcar"])
            for hf in range(2):
                (wv_,), wkey = acquire(f"w_out{l}_{hf}")
                for s in range(NSUB):
                    bi = bank("mm")
                    for kc in range(KC):
                        P.op("pe", lambda e, kc=kc: e.matmul(psb[bi][:], lhsT=mixT[:, kc, s * 128:(s + 1) * 128],
                                                             rhs=wv_[:, kc, :], start=(kc == 0), stop=(kc == KC - 1)),
                             reads=[wkey, "mixT"], writes=[pk(bi)])
                    resid_add(s, hf, bi)

        def xattn(l, t):
            norm([(xres[:, s, :], xkh(s)) for s in range(NSUB)], 3 * l + 1, "T")
            (wv_,), wkey = acquire(f"xa_wq{l}")
            for c in range(4):
                proj_fm(wv_, wkey, c, T, "act" if c % 2 == 0 else "dve", qT[:, c, :], "qT")
            sc_ = 1.0 / math.sqrt(128.0)
            for b0 in range(0, NSUB, 2):
                ba = [bank("att4") for _ in range(4)]
                for v in range(8):
                    bb, h = v // 4, v % 4
                    b = b0 + bb
                    bi = ba[v // 2]
                    P.op("pe", lambda e, h=h, bi=bi, v=v, b=b: e.matmul(psb[bi][:, (v % 2) * 256:(v % 2 + 1) * 256],
                                                                     lhsT=qT[:, h, b * 128:(b + 1) * 128], rhs=kmT[:, l, h, :],
                                                                     start=True, stop=True),
                         reads=["qT", "kmT"], writes=[pk(bi)])
                for pr in range(4):
                    P.op("dve", lambda e, pr=pr: e.tensor_scalar(out=Lb[:, 2 * pr:2 * pr + 2, :],
                                                                 in0=psb[ba[pr]][:].rearrange("p (h k) -> p h k", h=2),
                                                                 scalar1=sc_, scalar2=None, op0=ALU.mult),
                         reads=[pk(ba[pr])], writes=["Lb"])
                softmax_block(8, None)
                for bb in range(2):
                    b = b0 + bb
                    bo = bank("mm")
                    for h in range(4):
                        v = bb * 4 + h
                        for kb in range(2):
                            P.op("pe", lambda e, h=h, kb=kb, v=v: e.matmul(psb[bo][:, h * 128:(h + 1) * 128],
                                                                          lhsT=vm[:, l, kb, h * 128:(h + 1) * 128],
                                                                          rhs=PT[:, v, kb, :], start=(kb == 0), stop=(kb == 1)),
                                 reads=["PT", "vm"], writes=[pk(bo)])
                    evac("dve" if bb == 0 else "act", mixT[:, 0:4, b * 128:(b + 1) * 128],
                         psb[bo][:].rearrange("p (c n) -> p c n", c=4), [pk(bo)], ["mixT"])
            (wo_,), wkey = acquire(f"xa_wo{l}")
            for hf in range(2):
                for s in range(NSUB):
                    bi = bank("mm")
                    for kc in range(4):
                        P.op("pe", lambda e, kc=kc: e.matmul(psb[bi][:], lhsT=mixT[:, kc, s * 128:(s + 1) * 128],
                                                             rhs=wo_[:, kc, hf * 512:(hf + 1) * 512],
                                                             start=(kc == 0), stop=(kc == 3)),
                             reads=[wkey, "mixT"], writes=[pk(bi)])
                    resid_add(s, hf, bi)

        fstate = dict(n=0)

        def ffn(l, t):
            norm([(xres[:, s, :], xkh(s)) for s in range(NSUB)], 3 * l + 2, "T", router=(l == 1))
            ne_, ng_ = (1, DFF // 256) if l == 0 else (nexp, DFE // 256)
            groups = [(e_, gi) for e_ in range(ne_) for gi in range(ng_)]

            def up(e_, gi):
                name = f"dense_{gi}" if l == 0 else f"moe{e_}_{gi}"
                (wg_, wu_, wd_), wkey = acquire(name)
                hj = fstate["n"] % 2
                fstate["n"] += 1
                hb, hk = h1[hj], f"h1{hj}"
                for cc in range(2):
                    bg, bu = bank("mm"), bank("mm")
                    for kc in range(KC):
                        P.op("pe", lambda e, kc=kc: e.matmul(psb[bg][:], lhsT=wg_[:, kc, cc * 128:(cc + 1) * 128],
                                                             rhs=hT[:, kc, :], start=(kc == 0), stop=(kc == KC - 1)),
                             reads=[wkey, "hT"], writes=[pk(bg)])
                    for kc in range(KC):
                        P.op("pe", lambda e, kc=kc: e.matmul(psb[bu][:], lhsT=wu_[:, kc, cc * 128:(cc + 1) * 128],
                                                             rhs=hT[:, kc, :], start=(kc == 0), stop=(kc == KC - 1)),
                             reads=[wkey, "hT"], writes=[pk(bu)])
                    sj = cc
                    P.op("act", lambda e: e.activation(out=sil[sj][:], in_=psb[bg][:], func=AF.Silu),
                         reads=[pk(bg)], writes=[f"sil{sj}"])
                    P.op("dve", lambda e: e.tensor_tensor(out=hb[:, cc, :], in0=sil[sj][:], in1=psb[bu][:], op=ALU.mult),
                         reads=[f"sil{sj}", pk(bu)], writes=[f"{hk}_{cc}"])
                return (hb, hk, wd_, wkey, e_)

            def down(stt):
                hb, hk, wd_, wkey, e_ = stt
                for s in range(NSUB):
                    for hf in range(2):
                        bi = bank("dn")
                        for cc in range(2):
                            P.op("pe", lambda e, cc=cc: e.matmul(psb[bi][:], lhsT=hb[:, cc, s * 128:(s + 1) * 128],
                                                                 rhs=wd_[:, cc, hf * 512:(hf + 1) * 512],
                                                                 start=(cc == 0), stop=(cc == 1)),
                                 reads=[wkey, f"{hk}_{cc}"], writes=[pk(bi)])
                        resid_add(s, hf, bi, None if l == 0 else gates[:, s, e_:e_ + 1])

            prev = None
            for gidx, (e_, gi) in enumerate(groups):
                cur = up(e_, gi)
                if t == 0 and l == 0 and cast_pending and cast_pending[0][0] == "E0":
                    cast_tick()
                if t == 0 and l == 1 and ((gidx + 1) * 4) // 7 > (gidx * 4) // 7:
                    cast_tick()
                if prev is not None:
                    down(prev)
                prev = cur
            down(prev)
            if t == 0 and l == 1:
                cast_tick(len(cast_pending))

        for t in range(NT):
            for s_ in range(NSUB):
                P.dma("sp", [(xres[:, s_, :], x_d[t * T + s_ * 128:t * T + (s_ + 1) * 128, :])], f"xld{s_}",
                      writes=xkh(s_))
            for l in range(2):
                st = l * 3
                if st < stop_after:
                    mixer(l, t)
                if t == 0 and l == 1:
                    cast_experts()
                if st + 1 < stop_after:
                    xattn(l, t)
                if st + 2 < stop_after:
                    ffn(l, t)
            norm([(xres[:, s, :], xkh(s)) for s in range(NSUB)], 6, "store", ybase=t * T)
        assert wstate["next"] == len(sched), (wstate, len(sched))
        for j in range(2):
            k = f"ost{j}"
            if k in P.sems:
                nc.sync.wait_ge(P.sems[k], P.cnt[k])
    return nc


def _t5_bucket(dist):
    n = np.maximum(dist, 0)
    large = 16 + (np.log(np.maximum(n, 1).astype(np.float32) / 16) / math.log(128 / 16) * 16).astype(np.int32)
    large = np.minimum(large, 31)
    return np.where(n < 16, n, large)


def prepare_shared(inp):
    f = lambda a: np.ascontiguousarray(np.asarray(a, dtype=np.float32))
    w_in = f(inp["w_in"])
    q, k, v = w_in[:, :, 0:512], w_in[:, :, 512:640], w_in[:, :, 640:768]
    Bc, Cc, Xc = w_in[:, :, 768:1024], w_in[:, :, 1024:1280], w_in[:, :, 1280:1536]
    Rc, Gc = w_in[:, :, 1536:1792], w_in[:, :, 1792:2048]
    cols = [q, k[:, :, 0:64], k[:, :, 0:64], k[:, :, 64:128], k[:, :, 64:128]]
    for i in range(2):
        cols += [Bc[:, :, i * 128:(i + 1) * 128], Cc[:, :, i * 128:(i + 1) * 128], Xc[:, :, i * 128:(i + 1) * 128]]
    for i in range(2):
        cols += [Rc[:, :, i * 128:(i + 1) * 128], Gc[:, :, i * 128:(i + 1) * 128]]
    cols += [v]
    w_inp = np.ascontiguousarray(np.concatenate(cols, axis=2))
    assert w_inp.shape[2] == INC
    gains = np.concatenate([
        np.stack([f(inp["mix_norm"])[0], f(inp["xa_norm"])[0], f(inp["ffn_norm"])[0],
                  f(inp["mix_norm"])[1], f(inp["xa_norm"])[1], f(inp["ffn_norm"])[1],
                  f(inp["final_norm"])], 0),
        f(inp["mem_norm"])], 0)
    qi = np.arange(128)[:, None]
    ki = np.arange(256)[None, :]
    dist = qi + 128 - ki
    bucket = _t5_bucket(dist)
    rel = f(inp["rel_bias"])
    biasg = np.ascontiguousarray(rel[bucket].transpose(0, 2, 1))
    maskc = np.where((dist >= 0) & (dist < 128), 0.0, NEG).astype(np.float32)
    chanp = np.zeros((128, 48), np.float32)
    for l in range(2):
        for i in range(2):
            sl = slice(i * 128, (i + 1) * 128)
            cb = 24 * l + i * 4
            chanp[:, cb:cb + 3] = f(inp["sc_conv_w"])[l][:, sl].T
            chanp[:, cb + 3] = f(inp["sc_conv_b"])[l][sl]
            cb = 24 * l + 8 + i * 5
            chanp[:, cb:cb + 4] = f(inp["rg_conv_w"])[l][:, sl].T
            chanp[:, cb + 4] = f(inp["rg_conv_b"])[l][sl]
            cg = 24 * l + 18 + i * 3
            chanp[:, cg] = f(inp["rg_b_a"])[l][sl]
            chanp[:, cg + 1] = f(inp["rg_b_x"])[l][sl]
            chanp[:, cg + 2] = f(inp["rg_lambda"])[l][sl]
    rgw = np.ascontiguousarray(np.stack([f(inp["rg_w_a"]), f(inp["rg_w_x"])], 1))
    shared = {
        "w_inp": w_inp, "w_out": f(inp["w_out"]), "xa_wq": f(inp["xa_wq"]), "xa_wk": f(inp["xa_wk"]),
        "xa_wv": f(inp["xa_wv"]), "xa_wo": f(inp["xa_wo"]),
        "dense_wg": f(inp["dense_wg"]), "dense_wu": f(inp["dense_wu"]), "dense_wd": f(inp["dense_wd"]),
        "moe_wg": f(inp["moe_wg"])[0], "moe_wu": f(inp["moe_wu"])[0], "moe_wd": f(inp["moe_wd"])[0],
        "router": f(inp["moe_router"])[0], "gains": np.ascontiguousarray(gains),
        "biasg": biasg, "maskc": maskc, "sinks": f(inp["attn_sinks"]).reshape(1, 16),
        "chanp": chanp, "rgw": rgw, "ident": np.eye(128, dtype=np.float32),
    }
    return shared


def kernel(**inputs):
    shared = prepare_shared(inputs)
    x = np.asarray(inputs["x"], dtype=np.float32)
    mem = np.asarray(inputs["mem"], dtype=np.float32)
    nc = bass.Bass("TRN2", target_bir_lowering=False)
    build_program(nc, SEQ // T)
    in_maps = []
    for c in range(NCORES):
        m = dict(shared)
        m["x"] = np.ascontiguousarray(x[c])
        m["mem"] = np.ascontiguousarray(mem[c])
        in_maps.append(m)
    res = run_bass_kernel_spmd(nc, in_maps, core_ids=list(range(NCORES)))
    return np.stack([np.asarray(r["y"], dtype=np.float32) for r in res.results], 0)
```

```python
import contextlib
import math
import numpy as np
import concourse.bass as bass
import concourse.mybir as mybir
from concourse.bass_utils import run_bass_kernel_spmd

F32 = mybir.dt.float32
BF16 = mybir.dt.bfloat16
ALU = mybir.AluOpType
AF = mybir.ActivationFunctionType
AX = mybir.AxisListType

D = 1024
KC = 8
T = 512
NSUB = 4
SEQ = 4096
NCORES = 8
INC = 2176
DFF = 2816
DFE = 3584
NE = 8
SLOT = 6144
NSLOT = 4
NEG = -1.0e30
SELF_SYNC = True
SELF_WAR = False
DEBUG = {"att": True, "sc": True, "rg": True, "wout": True}


class Prog:
    def __init__(self, nc, es):
        self.nc = nc
        self.es = es
        self.engs = {"pe": nc.tensor, "act": nc.scalar, "dve": nc.vector, "pool": nc.gpsimd, "sp": nc.sync}
        self.sems = {}
        self.cnt = {}
        for n in self.engs:
            self.sems[n] = es.enter_context(nc.semaphore("sem_" + n))
            self.cnt[n] = 0
        self.waited = {n: {} for n in self.engs}
        self.lastw = {}
        self.readers = {}

    def new_sem(self, key):
        if key not in self.sems:
            self.sems[key] = self.es.enter_context(self.nc.semaphore("sem_" + key.replace(":", "_")))
            self.cnt[key] = 0

    def _deps(self, reads, writes, extra=()):
        deps = {}

        def add(d):
            if d is not None:
                deps[d[0]] = max(deps.get(d[0], 0), d[1])

        for k in reads:
            add(self.lastw.get(k))
        for k in writes:
            add(self.lastw.get(k))
        self._raw_self = dict(deps)
        for k in writes:
            for sk, v in self.readers.get(k, {}).items():
                add((sk, v))
        for d in extra:
            add(d)
        return deps

    def _wait(self, x, deps):
        for sk, v in deps.items():
            if sk == x and (x == "pe" or not SELF_SYNC):
                continue
            if sk == x and not SELF_WAR:
                v = self._raw_self.get(x, 0)
                if v == 0:
                    continue
            if self.waited[x].get(sk, 0) >= v:
                continue
            self.engs[x].wait_ge(self.sems[sk], v)
            self.waited[x][sk] = v

    def op(self, x, fn, reads=(), writes=()):
        self._wait(x, self._deps(reads, writes))
        ins = fn(self.engs[x])
        self.cnt[x] += 1
        ins.then_inc(self.sems[x], 1)
        c = self.cnt[x]
        for k in reads:
            self.readers.setdefault(k, {})[x] = c
        for k in writes:
            self.lastw[k] = (x, c)
            self.readers[k] = {}

    def dma(self, q, pairs, semkey, reads=(), writes=(), extra=()):
        self.new_sem(semkey)
        self._wait(q, self._deps(reads, writes, extra))
        for out, in_ in pairs:
            ins = self.engs[q].dma_start(out=out, in_=in_)
            ins.then_inc(self.sems[semkey], 16)
            self.cnt[semkey] += 16
        c = self.cnt[semkey]
        for k in reads:
            self.readers.setdefault(k, {})[semkey] = c
        for k in writes:
            self.lastw[k] = (semkey, c)
            self.readers[k] = {}


def build_program(nc, NT, stop_after=99, nexp=NE):
    S = NT * T
    es = contextlib.ExitStack()
    with es:
        P = Prog(nc, es)

        def din(name, shape, dt=F32):
            return nc.dram_tensor(name, list(shape), dt, kind="ExternalInput").ap()

        def dint(name, shape, dt=BF16):
            return nc.dram_tensor(name, list(shape), dt, kind="Internal").ap()

        x_d = din("x", [S, D])
        mem_d = din("mem", [256, D])
        y_d = nc.dram_tensor("y", [S, D], F32, kind="ExternalOutput").ap()
        w = {}
        wshapes = {
            "w_inp": [2, D, INC], "w_out": [2, D, D], "xa_wq": [2, D, 512], "xa_wk": [2, D, 512],
            "xa_wv": [2, D, 512], "xa_wo": [2, 512, D], "dense_wg": [1, D, DFF], "dense_wu": [1, D, DFF],
            "dense_wd": [1, DFF, D], "moe_wg": [NE, D, DFE], "moe_wu": [NE, D, DFE], "moe_wd": [NE, DFE, D],
        }
        w16 = {}
        for k, shp in wshapes.items():
            w[k] = din(k, shp)
            w16[k] = dint(k + "_16", shp)
        router_d = din("router", [D, NE])
        gains_d = din("gains", [9, D])
        biasg_d = din("biasg", [128, 8, 256])
        maskc_d = din("maskc", [128, 256])
        sinks_d = din("sinks", [1, 16])
        chanp_d = din("chanp", [128, 48])
        rgw_d = din("rgw", [2, 2, 4, 64, 64])
        ident_d = din("ident", [128, 128])

        def sb(name, shape, dt=F32):
            return es.enter_context(nc.sbuf_tensor(name, list(shape), dt))

        xres = sb("xres", [128, NSUB, D])
        bias8 = sb("bias8", [128, 8, 256])
        kdup = sb("kdup", [128, 2, 2, 128 + T], BF16)
        vpad = sb("vpad", [128, 5, 2, 2, 128], BF16)
        kcar = sb("kcar", [128, 2, 4, 128], BF16)
        vcar = sb("vcar", [128, 2, 4, 128], BF16)
        ucar = sb("ucar", [128, 2, 2, 2])
        xcar = sb("xcar", [128, 2, 2, 3])
        hcar = sb("hcar", [128, 2, 2, 1])
        kmT = sb("kmT", [128, 2, 4, 256], BF16)
        vm = sb("vm", [128, 2, 2, 512], BF16)
        ident32 = sb("ident32", [128, 128])
        identb = sb("identb", [128, 128], BF16)
        chanp = sb("chanp_sb", [128, 48])
        dcp = sb("dcp", [128, 2, 2, 2])
        sinks = sb("sinks_sb", [128, 16])
        BD = sb("BD", [128, 2, 2, 2, 128], BF16)
        router_sb = sb("router_sb", [128, KC, NE])
        maskc = sb("maskc_sb", [128, 256])
        small = sb("small", [128, 128])
        xn = [sb("xn0", [128, D]), sb("xn1", [128, D])]
        gbc = [sb("gbc0", [128, D]), sb("gbc1", [128, D])]
        hT = sb("hT", [128, KC, T], BF16)
        hT32 = sb("hT32", [128, KC, 128])
        qT = sb("qT", [128, 4, T], BF16)
        NBR = 5
        br = [sb(f"br{i}", [128, T]) for i in range(NBR)]
        ubuf = sb("ubuf", [128, 2 + T])
        xbuf = sb("xbuf", [128, 3 + T])
        tA = sb("tA", [128, T])
        tB = sb("tB", [128, T])
        tC = sb("tC", [128, T])
        tD = sb("tD", [128, T])
        rg16 = sb("rg16", [128, T], BF16)
        Lb = sb("Lb", [128, 8, 256])
        Pn = sb("Pn", [128, 8, 256], BF16)
        PT = sb("PT", [128, 8, 2, 128], BF16)
        mixT = sb("mixT", [128, KC, T], BF16)
        h1 = [sb("h1a", [128, 2, T], BF16), sb("h1b", [128, 2, T], BF16)]
        sil = [sb("sil0", [128, T]), sb("sil1", [128, T])]
        gates = sb("gates", [128, NSUB, NE])
        slots = [sb(f"slot{i}", [128, SLOT], BF16) for i in range(NSLOT)]
        psb = [es.enter_context(nc.psum_tensor(f"ps{i}", [128, 512], F32)) for i in range(8)]

        _sc = [0]

        def sm(n, key):
            a = small[:, _sc[0]:_sc[0] + n]
            _sc[0] += n
            assert _sc[0] <= 128
            return a

        epsc = sm(1, "c")
        onec = sm(1, "c")
        ss = sm(1, "ss")
        rs = sm(1, "rs")
        rstd = sm(1, "rstd")
        mx = sm(8, "mx")
        negm = sm(8, "negm")
        rsum = sm(8, "rsum")
        dsk = sm(8, "dsk")
        rec = sm(8, "rec")
        ss4 = sm(4, "ss4")
        rs4 = sm(4, "rs4")
        rstd4 = sm(4, "rstd4")
        lg = sm(8, "lg")
        top8 = sm(8, "top8")
        nm1 = sm(1, "nm1")
        sel = sm(8, "sel")
        ex = sm(8, "ex")
        gsm = sm(1, "gsm")
        grc = sm(1, "grc")
        lam_t = sm(8, "lam")

        rot = {"mm": [0, 1, 2, 3], "tr": [4, 5], "att": [6, 7], "dn": [4, 5, 6, 7], "att4": [4, 5, 6, 7]}
        rpos = {"mm": 0, "tr": 0, "att": 0, "dn": 0, "att4": 0}

        def bank(role):
            i = rot[role][rpos[role] % len(rot[role])]
            rpos[role] += 1
            return i

        def pk(i):
            return f"ps{i}"

        P.new_sem("const")
        cpairs = [
            (ident32[:], ident_d), (chanp[:], chanp_d), (maskc[:], maskc_d), (bias8[:], biasg_d),
            (sinks[:], sinks_d[0, :].partition_broadcast(128)),
            (router_sb[:], router_d.rearrange("(k p) e -> p k e", p=128)),
        ]
        ckeys = ["ident32", "chanp", "maskc", "bias8", "sinks", "router"]
        P.dma("sp", cpairs, "const", writes=ckeys)

        def pset(ap, val, key):
            P.op("pool", lambda e: e.memset(ap, val), writes=[key])

        pset(BD[:], 0.0, "BD")
        pset(vpad[:], 0.0, "vpad_all")
        pset(kdup[:], 0.0, "kdup")
        pset(kcar[:], 0.0, "kcar")
        pset(vcar[:], 0.0, "vcar")
        pset(ucar[:], 0.0, "ucar")
        pset(xcar[:], 0.0, "xcar")
        pset(hcar[:], 0.0, "hcar")
        pset(epsc, 1e-6, "c")
        pset(onec, 1.0, "c")
        bdpairs = []
        for l in range(2):
            for ax in range(2):
                for hh in range(4):
                    i, o = hh // 2, (hh % 2) * 64
                    bdpairs.append((BD[o:o + 64, l, ax, i, o:o + 64], rgw_d[l, ax, hh]))
        P.dma("pool", bdpairs, "bdld", writes=["BD"])

        cast_total = {}

        cast_tot = {}
        cast_pending = []
        cast_issued = []

        def cast_issue(item):
            grp, d_, s_ = item
            nc.gpsimd.dma_start(out=d_, in_=s_).then_inc(P.sems["cast:" + grp], 16)
            P.cnt["cast:" + grp] += 16
            cast_issued.append(("cast:" + grp, P.cnt["cast:" + grp]))

        def cast_tick(n=1):
            for _ in range(n):
                if not cast_pending:
                    return
                nc.gpsimd.wait_ge(P.sems["pe"], P.cnt["pe"])
                if len(cast_issued) >= 2:
                    sk, v = cast_issued[-2]
                    nc.gpsimd.wait_ge(P.sems[sk], v)
                cast_issue(cast_pending.pop(0))

        def cast2d(src, dst, grp, defer=False):
            P.new_sem("cast:" + grp)
            R, C = src.shape
            ns = (C + 2047) // 2048
            assert C % ns == 0
            RB = 1024 if C <= 2048 else 512
            for r0 in range(0, R, RB):
                r1 = min(R, r0 + RB)
                s_ = src[r0:r1, :].rearrange("r (a c) -> r a c", a=ns)
                d_ = dst[r0:r1, :].rearrange("r (a c) -> r a c", a=ns)
                cast_tot[grp] = cast_tot.get(grp, 0) + 16
                if defer:
                    cast_pending.append((grp, d_, s_))
                else:
                    cast_issue((grp, d_, s_))

        def cast_fence(grp):
            nc.gpsimd.wait_ge(P.sems["cast:" + grp], P.cnt["cast:" + grp])

        for l in range(2):
            cast2d(w["xa_wk"][l], w16["xa_wk"][l], "A")
            cast2d(w["xa_wv"][l], w16["xa_wv"][l], "A")
        for k in ["w_inp", "w_out", "xa_wq", "xa_wo"]:
            cast2d(w[k][0], w16[k][0], "A")
        cast_fence("A")
        for k in ["dense_wg", "dense_wu", "dense_wd"]:
            cast2d(w[k][0], w16[k][0], "B")
        for k in ["w_inp", "w_out", "xa_wq", "xa_wo"]:
            cast2d(w[k][1], w16[k][1], "C")
        cast_fence("B")
        for e in range(nexp):
            P.new_sem(f"cast:E{e}")

        def cast_experts():
            cast_tick(len([1 for it in cast_pending if it[0] == "E0"]))

        for e in range(nexp):
            for k in ["moe_wg", "moe_wu", "moe_wd"]:
                cast2d(w[k][e], w16[k][e], f"E{e}", defer=True)

        def castdep(grp):
            return ("cast:" + grp, cast_tot[grp])

        def kview(slot, off, k, n):
            return slot[:, off:off + k * n].rearrange("p (k n) -> p k n", k=k)

        def piece_cols(name, src2d, c0, c1, grp, k=KC):
            n = c1 - c0
            return dict(name=name, grp=grp,
                        parts=[(lambda s, k=k, n=n: kview(s, 0, k, n),
                                src2d[:, c0:c1].rearrange("(k p) n -> p k n", p=128))])

        def piece_ffn(name, wg, wu, wd, gi, grp):
            f0 = gi * 256
            return dict(name=name, grp=grp, parts=[
                (lambda s: kview(s, 0, KC, 256), wg[:, f0:f0 + 256].rearrange("(k p) n -> p k n", p=128)),
                (lambda s: kview(s, 2048, KC, 256), wu[:, f0:f0 + 256].rearrange("(k p) n -> p k n", p=128)),
                (lambda s: kview(s, 4096, 2, D), wd[f0:f0 + 256, :].rearrange("(c p) n -> p c n", p=128)),
            ])

        sched = []
        for l in range(2):
            sched.append(piece_cols(f"xa_wk{l}", w16["xa_wk"][l], 0, 512, "A"))
            sched.append(piece_cols(f"xa_wv{l}", w16["xa_wv"][l], 0, 512, "A"))
        NSTAGE = 0
        for t in range(NT):
            for l in range(2):
                g = "A" if l == 0 else "C"
                st = l * 3
                if st < stop_after:
                    for pc in range(5):
                        c0, c1 = pc * 512, min(INC, pc * 512 + 512)
                        sched.append(piece_cols(f"w_in{l}_{pc}", w16["w_inp"][l], c0, c1, g))
                    for hf in range(2):
                        sched.append(piece_cols(f"w_out{l}_{hf}", w16["w_out"][l], hf * 512, hf * 512 + 512, g))
                if st + 1 < stop_after:
                    sched.append(piece_cols(f"xa_wq{l}", w16["xa_wq"][l], 0, 512, g))
                    sched.append(piece_cols(f"xa_wo{l}", w16["xa_wo"][l], 0, D, g, k=4))
                if st + 2 < stop_after:
                    if l == 0:
                        for gi in range(DFF // 256):
                            sched.append(piece_ffn(f"dense_{gi}", w16["dense_wg"][0], w16["dense_wu"][0],
                                                   w16["dense_wd"][0], gi, "B"))
                    else:
                        for e in range(nexp):
                            for gi in range(DFE // 256):
                                sched.append(piece_ffn(f"moe{e}_{gi}", w16["moe_wg"][e], w16["moe_wu"][e],
                                                       w16["moe_wd"][e], gi, f"E{e}"))
        wstate = dict(issued=0, next=0)

        def acquire(name):
            i = wstate["next"]
            assert sched[i]["name"] == name, (sched[i]["name"], name)
            while wstate["issued"] < min(len(sched), i + NSLOT - 1):
                j = wstate["issued"]
                pcs = sched[j]
                sl = slots[j % NSLOT]
                P.dma("sp", [(mk(sl), src) for mk, src in pcs["parts"]], f"slot{j % NSLOT}",
                      writes=[f"slot{j % NSLOT}"], extra=[castdep(pcs["grp"])])
                wstate["issued"] += 1
            wstate["next"] += 1
            sl = slots[i % NSLOT]
            return [mk(sl) for mk, _ in sched[i]["parts"]], f"slot{i % NSLOT}"

        for l in range(2):
            for i in range(2):
                col = 24 * l + 18 + 3 * i + 2
                j = (l * 2 + i) * 2
                P.op("act", lambda e, col=col, j=j: e.activation(out=lam_t[:, j:j + 1], in_=chanp[:, col:col + 1],
                                                                 func=AF.Exp, scale=-1.0),
                     reads=["chanp"], writes=["lam"])
                P.op("act", lambda e, j=j: e.activation(out=lam_t[:, j + 1:j + 2], in_=lam_t[:, j:j + 1],
                                                        func=AF.Ln, bias=onec, scale=1.0),
                     reads=["lam", "c"], writes=["lam"])
                P.op("dve", lambda e, l=l, i=i, j=j: e.tensor_scalar(out=dcp[:, l, i, 0:1], in0=lam_t[:, j + 1:j + 2],
                                                                     scalar1=-8.0, scalar2=None, op0=ALU.mult),
                     reads=["lam"], writes=["dcp"])
                P.op("dve", lambda e, l=l, i=i, j=j: e.tensor_scalar(out=dcp[:, l, i, 1:2], in0=lam_t[:, j + 1:j + 2],
                                                                     scalar1=-16.0, scalar2=None, op0=ALU.mult),
                     reads=["lam"], writes=["dcp"])
        for h in range(8):
            P.op("dve", lambda e, h=h: e.tensor_tensor(out=bias8[:, h, :], in0=bias8[:, h, :], in1=maskc[:], op=ALU.add),
                 reads=["bias8", "maskc"], writes=["bias8"])
        P.op("dve", lambda e: e.tensor_copy(out=identb[:], in_=ident32[:]), reads=["ident32"], writes=["identb"])

        gstate = dict(g=0, n=0)

        def load_gain(row):
            j = gstate["g"] % 2
            gstate["g"] += 1
            P.dma("sp", [(gbc[j][:], gains_d[row, :].partition_broadcast(128))], f"gbc{j}", writes=[f"gbc{j}"])
            return j

        def evac(eng, out, in_, reads, writes):
            if eng == "act":
                P.op("act", lambda e: e.activation(out=out, in_=in_, func=AF.Copy), reads=reads, writes=writes)
            else:
                P.op("dve", lambda e: e.tensor_copy(out=out, in_=in_), reads=reads, writes=writes)

        def norm(srcs, row, mode, col0=0, router=False, ybase=None):
            gj = load_gain(row)
            ns_ = len(srcs)
            junk = Lb[:].rearrange("p h k -> p (h k)")
            for si, (xa, xkey) in enumerate(srcs):
                xkeys = list(xkey) if isinstance(xkey, (list, tuple)) else [xkey]
                P.op("act", lambda e: e.activation(out=junk[:, (si % 2) * D:(si % 2 + 1) * D], in_=xa, func=AF.Square,
                                                   accum_out=ss4[:, si:si + 1]),
                     reads=xkeys, writes=["Lb", "ss4"])
            P.op("act", lambda e: e.activation(out=rs4[:, 0:ns_], in_=ss4[:, 0:ns_], func=AF.Sqrt, bias=epsc, scale=1.0 / D),
                 reads=["ss4", "c"], writes=["rs4"])
            P.op("dve", lambda e: e.reciprocal(out=rstd4[:, 0:ns_], in_=rs4[:, 0:ns_]), reads=["rs4"], writes=["rstd4"])
            for si, (xa, xkey) in enumerate(srcs):
                j = gstate["n"] % 2
                gstate["n"] += 1
                xb = xn[j]
                xk = f"xn{j}"
                xkeys = list(xkey) if isinstance(xkey, (list, tuple)) else [xkey]
                P.op("dve", lambda e: e.scalar_tensor_tensor(out=xb[:], in0=xa, scalar=rstd4[:, si:si + 1], in1=gbc[gj][:],
                                                             op0=ALU.mult, op1=ALU.mult),
                     reads=xkeys + ["rstd4", f"gbc{gj}"], writes=[xk])
                if mode == "store":
                    P.dma("act", [(y_d[ybase + si * 128: ybase + (si + 1) * 128, :], xb[:])], f"ost{j}", reads=[xk])
                    continue
                b0, b1 = bank("tr"), bank("tr")
                for kc in range(KC):
                    bi = b0 if kc < 4 else b1
                    P.op("pe", lambda e, kc=kc, bi=bi: e.transpose(out=psb[bi][:, (kc % 4) * 128:(kc % 4 + 1) * 128],
                                                                  in_=xb[:, kc * 128:(kc + 1) * 128], identity=ident32[:]),
                         reads=[xk, "ident32"], writes=[pk(bi)])
                c = col0 + si * 128
                evac("act", hT[:, 0:4, c:c + 128], psb[b0][:].rearrange("p (k n) -> p k n", k=4), [pk(b0)], ["hT"])
                evac("dve", hT[:, 4:8, c:c + 128], psb[b1][:].rearrange("p (k n) -> p k n", k=4), [pk(b1)], ["hT"])
                if router:
                    evac("act", hT32[:, 0:4, :], psb[b0][:].rearrange("p (k n) -> p k n", k=4), [pk(b0)], ["hT32"])
                    evac("dve", hT32[:, 4:8, :], psb[b1][:].rearrange("p (k n) -> p k n", k=4), [pk(b1)], ["hT32"])
                    bl = bank("mm")
                    for kc in range(KC):
                        P.op("pe", lambda e, kc=kc: e.matmul(psb[bl][:, 0:NE], lhsT=hT32[:, kc, :], rhs=router_sb[:, kc, :],
                                                             start=(kc == 0), stop=(kc == KC - 1)),
                             reads=["hT32", "router"], writes=[pk(bl)])
                    P.op("dve", lambda e: e.tensor_copy(out=lg, in_=psb[bl][:, 0:NE]), reads=[pk(bl)], writes=["lg"])
                    P.op("dve", lambda e: e.max(out=top8, in_=lg), reads=["lg"], writes=["top8"])
                    P.op("dve", lambda e: e.tensor_scalar(out=nm1, in0=top8[:, 0:1], scalar1=-1.0, scalar2=None,
                                                          op0=ALU.mult), reads=["top8"], writes=["nm1"])
                    P.op("dve", lambda e: e.tensor_scalar(out=sel, in0=lg, scalar1=top8[:, 1:2], scalar2=None,
                                                          op0=ALU.is_ge), reads=["lg", "top8"], writes=["sel"])
                    P.op("act", lambda e: e.activation(out=ex, in_=lg, func=AF.Exp, bias=nm1, scale=1.0),
                         reads=["lg", "nm1"], writes=["ex"])
                    P.op("dve", lambda e: e.tensor_tensor(out=ex, in0=ex, in1=sel, op=ALU.mult),
                         reads=["ex", "sel"], writes=["ex"])
                    P.op("dve", lambda e: e.tensor_reduce(out=gsm, in_=ex, axis=AX.X, op=ALU.add),
                         reads=["ex"], writes=["gsm"])
                    P.op("dve", lambda e: e.reciprocal(out=grc, in_=gsm), reads=["gsm"], writes=["grc"])
                    P.op("dve", lambda e, si=si: e.tensor_scalar(out=gates[:, si, :], in0=ex, scalar1=grc, scalar2=None,
                                                                 op0=ALU.mult), reads=["ex", "grc"], writes=["gates"])

        def proj_fm(wv_, wkey, cidx, n_tok, evac_eng, out_ap, out_key, kcn=KC, rhs_cols=None):
            bi = bank("mm")
            c0, c1 = rhs_cols if rhs_cols else (0, n_tok)
            for kc in range(kcn):
                P.op("pe", lambda e, kc=kc: e.matmul(psb[bi][:, 0:n_tok], lhsT=wv_[:, kc, cidx * 128:(cidx + 1) * 128],
                                                     rhs=hT[:, kc, c0:c1], start=(kc == 0), stop=(kc == kcn - 1)),
                     reads=[wkey, "hT"], writes=[pk(bi)])
            evac(evac_eng, out_ap, psb[bi][:, 0:n_tok], [pk(bi)], [out_key])

        def softmax_block(nh, sink_ap):
            P.op("dve", lambda e: e.tensor_reduce(out=mx[:, 0:nh], in_=Lb[:, 0:nh, :], axis=AX.X, op=ALU.max),
                 reads=["Lb"], writes=["mx"])
            if sink_ap is not None:
                P.op("dve", lambda e: e.tensor_tensor(out=mx[:, 0:nh], in0=mx[:, 0:nh], in1=sink_ap, op=ALU.max),
                     reads=["mx", "sinks"], writes=["mx"])
            P.op("dve", lambda e: e.tensor_scalar(out=negm[:, 0:nh], in0=mx[:, 0:nh], scalar1=-1.0, scalar2=None,
                                                  op0=ALU.mult), reads=["mx"], writes=["negm"])
            for hh in range(nh):
                P.op("act", lambda e, hh=hh: e.activation(out=Lb[:, hh, :], in_=Lb[:, hh, :], func=AF.Exp,
                                                          bias=negm[:, hh:hh + 1], scale=1.0,
                                                          accum_out=rsum[:, hh:hh + 1]),
                     reads=["Lb", "negm"], writes=["Lb", "rsum"])
            if sink_ap is not None:
                P.op("dve", lambda e: e.tensor_tensor(out=dsk[:, 0:nh], in0=sink_ap, in1=mx[:, 0:nh], op=ALU.subtract),
                     reads=["mx", "sinks"], writes=["dsk"])
                P.op("act", lambda e: e.activation(out=dsk[:, 0:nh], in_=dsk[:, 0:nh], func=AF.Exp),
                     reads=["dsk"], writes=["dsk"])
                P.op("dve", lambda e: e.tensor_tensor(out=rsum[:, 0:nh], in0=rsum[:, 0:nh], in1=dsk[:, 0:nh], op=ALU.add),
                     reads=["rsum", "dsk"], writes=["rsum"])
            P.op("dve", lambda e: e.reciprocal(out=rec[:, 0:nh], in_=rsum[:, 0:nh]), reads=["rsum"], writes=["rec"])
            for hh in range(nh):
                if hh % 2 == 0:
                    P.op("dve", lambda e, hh=hh: e.tensor_scalar(out=Pn[:, hh, :], in0=Lb[:, hh, :],
                                                                 scalar1=rec[:, hh:hh + 1], scalar2=None, op0=ALU.mult),
                         reads=["Lb", "rec"], writes=[f"Pn{hh}"])
                else:
                    P.op("act", lambda e, hh=hh: e.activation(out=Pn[:, hh, :], in_=Lb[:, hh, :], func=AF.Copy,
                                                              scale=rec[:, hh:hh + 1]),
                         reads=["Lb", "rec"], writes=[f"Pn{hh}"])
            nb_ = (nh + 3) // 4
            bts = [bank("mm") for _ in range(nb_)]
            for hh in range(nh):
                bt = bts[hh // 4]
                pv = psb[bt][:].bitcast(BF16)
                for kb in range(2):
                    o = ((hh % 4) * 2 + kb) * 128
                    P.op("pe", lambda e, hh=hh, kb=kb, o=o, pv=pv: e.transpose(out=pv[:, o:o + 128],
                                                                              in_=Pn[:, hh, kb * 128:(kb + 1) * 128],
                                                                              identity=identb[:]),
                         reads=[f"Pn{hh}", "identb"], writes=[pk(bt)])
            ptf = PT[:].rearrange("p h k n -> p (h k n)")
            for g_ in range(nb_):
                n_ = min(4, nh - 4 * g_) * 256
                evac("act" if g_ == 0 else "dve", ptf[:, g_ * 1024:g_ * 1024 + n_],
                     psb[bts[g_]][:].bitcast(BF16)[:, 0:n_], [pk(bts[g_])], ["PT"])

        P.dma("sp", [(xres[:, 0:2, :], mem_d.rearrange("(s p) d -> p s d", p=128))], "xld", writes=["x0", "x0b", "x1", "x1b"])
        for l in range(2):
            norm([(xres[:, 0, :], ["x0", "x0b"]), (xres[:, 1, :], ["x1", "x1b"])], 7 + l, "T")
            (wk_,), wkey = acquire(f"xa_wk{l}")
            for h in range(4):
                proj_fm(wk_, wkey, h, 256, "act" if h % 2 == 0 else "dve", kmT[:, l, h, :], "kmT")
            (wv_,), wkey = acquire(f"xa_wv{l}")
            for mt in range(2):
                bi = bank("mm")
                for kc in range(KC):
                    P.op("pe", lambda e, kc=kc: e.matmul(psb[bi][:], lhsT=hT[:, kc, mt * 128:(mt + 1) * 128],
                                                         rhs=wv_[:, kc, :], start=(kc == 0), stop=(kc == KC - 1)),
                         reads=[wkey, "hT"], writes=[pk(bi)])
                evac("act", vm[:, l, mt, :], psb[bi][:], [pk(bi)], ["vm"])

        xk_ = [f"x{s}" for s in range(NSUB)]
        xkh = lambda s: [f"x{s}", f"x{s}b"]
        xall = [k for s in range(NSUB) for k in xkh(s)]

        def resid_add(s, hf, bi, gate_ap=None):
            xa = xres[:, s, hf * 512:(hf + 1) * 512]
            if gate_ap is None:
                P.op("dve", lambda e: e.tensor_tensor(out=xa, in0=psb[bi][:], in1=xa, op=ALU.add),
                     reads=[pk(bi), xkh(s)[hf]], writes=[xkh(s)[hf]])
            else:
                P.op("dve", lambda e: e.scalar_tensor_tensor(out=xa, in0=psb[bi][:], scalar=gate_ap, in1=xa,
                                                             op0=ALU.mult, op1=ALU.add),
                     reads=[pk(bi), xkh(s)[hf], "gates"], writes=[xkh(s)[hf]])

        def sc_branch(l, i, kB, kC_, kX, B, C, X):
            cb = 24 * l + i * 4
            P.op("dve", lambda e: e.tensor_copy(out=ubuf[:, 0:2], in_=ucar[:, l, i, :]), reads=["ucar"], writes=["ubuf"])
            P.op("dve", lambda e: e.tensor_tensor(out=ubuf[:, 2:2 + T], in0=C, in1=X, op=ALU.mult),
                 reads=[kC_, kX], writes=["ubuf"])
            P.op("dve", lambda e: e.tensor_copy(out=ucar[:, l, i, :], in_=ubuf[:, T:T + 2]), reads=["ubuf"], writes=["ucar"])
            P.op("dve", lambda e: e.tensor_scalar(out=tA[:], in0=ubuf[:, 0:T], scalar1=chanp[:, cb:cb + 1],
                                                  scalar2=chanp[:, cb + 3:cb + 4], op0=ALU.mult, op1=ALU.add),
                 reads=["ubuf", "chanp"], writes=["tA"])
            for k in (1, 2):
                P.op("dve", lambda e, k=k: e.scalar_tensor_tensor(out=tA[:], in0=ubuf[:, k:k + T],
                                                                  scalar=chanp[:, cb + k:cb + k + 1], in1=tA[:],
                                                                  op0=ALU.mult, op1=ALU.add),
                     reads=["ubuf", "chanp", "tA"], writes=["tA"])
            P.op("dve", lambda e: e.tensor_tensor(out=mixT[:, 4 + i, :], in0=B, in1=tA[:], op=ALU.mult),
                 reads=[kB, "tA"], writes=["mixT"])

        def rg_branch(l, i, kR, kG, R, G):
            cb = 24 * l + 8 + i * 5
            cg = 24 * l + 18 + i * 3
            P.op("dve", lambda e: e.tensor_copy(out=xbuf[:, 0:3], in_=xcar[:, l, i, :]), reads=["xcar"], writes=["xbuf"])
            if R is not None:
                P.op("dve", lambda e: e.tensor_copy(out=xbuf[:, 3:3 + T], in_=R), reads=[kR], writes=["xbuf"])
            P.op("dve", lambda e: e.tensor_copy(out=xcar[:, l, i, :], in_=xbuf[:, T:T + 3]), reads=["xbuf"], writes=["xcar"])
            P.op("dve", lambda e: e.tensor_scalar(out=tA[:], in0=xbuf[:, 0:T], scalar1=chanp[:, cb:cb + 1],
                                                  scalar2=chanp[:, cb + 4:cb + 5], op0=ALU.mult, op1=ALU.add),
                 reads=["xbuf", "chanp"], writes=["tA"])
            for k in (1, 2, 3):
                P.op("dve", lambda e, k=k: e.scalar_tensor_tensor(out=tA[:], in0=xbuf[:, k:k + T],
                                                                  scalar=chanp[:, cb + k:cb + k + 1], in1=tA[:],
                                                                  op0=ALU.mult, op1=ALU.add),
                     reads=["xbuf", "chanp", "tA"], writes=["tA"])
            P.op("act", lambda e: e.activation(out=rg16[:], in_=tA[:], func=AF.Copy), reads=["tA"], writes=["rg16"])
            b_r, b_i = bank("mm"), bank("mm")
            P.op("pe", lambda e: e.matmul(psb[b_r][:], lhsT=BD[:, l, 0, i, :], rhs=rg16[:], start=True, stop=True),
                 reads=["BD", "rg16"], writes=[pk(b_r)])
            P.op("pe", lambda e: e.matmul(psb[b_i][:], lhsT=BD[:, l, 1, i, :], rhs=rg16[:], start=True, stop=True),
                 reads=["BD", "rg16"], writes=[pk(b_i)])
            P.op("act", lambda e: e.activation(out=tB[:], in_=psb[b_r][:], func=AF.Sigmoid, bias=chanp[:, cg:cg + 1], scale=1.0),
                 reads=[pk(b_r), "chanp"], writes=["tB"])
            P.op("act", lambda e: e.activation(out=tC[:], in_=psb[b_i][:], func=AF.Sigmoid, bias=chanp[:, cg + 1:cg + 2], scale=1.0),
                 reads=[pk(b_i), "chanp"], writes=["tC"])
            P.op("act", lambda e: e.activation(out=tD[:], in_=tB[:], func=AF.Exp, scale=dcp[:, l, i, 0:1]),
                 reads=["tB", "dcp"], writes=["tD"])
            P.op("act", lambda e: e.activation(out=tB[:], in_=tB[:], func=AF.Exp, scale=dcp[:, l, i, 1:2]),
                 reads=["tB", "dcp"], writes=["tB"])
            P.op("dve", lambda e: e.tensor_scalar(out=tB[:], in0=tB[:], scalar1=1.0, scalar2=-1.0, op0=ALU.min, op1=ALU.mult),
                 reads=["tB"], writes=["tB"])
            P.op("act", lambda e: e.activation(out=tB[:], in_=tB[:], func=AF.Sqrt, bias=onec, scale=1.0),
                 reads=["tB", "c"], writes=["tB"])
            P.op("dve", lambda e: e.tensor_tensor(out=tC[:], in0=tC[:], in1=tA[:], op=ALU.mult),
                 reads=["tC", "tA"], writes=["tC"])
            P.op("dve", lambda e: e.tensor_tensor(out=tC[:], in0=tC[:], in1=tB[:], op=ALU.mult),
                 reads=["tC", "tB"], writes=["tC"])
            P.op("dve", lambda e: e.tensor_tensor_scan(out=tA[:], data0=tD[:], data1=tC[:], initial=hcar[:, l, i, :],
                                                       op0=ALU.mult, op1=ALU.add),
                 reads=["tD", "tC", "hcar", "tA"], writes=["tA"])
            P.op("dve", lambda e: e.tensor_copy(out=hcar[:, l, i, :], in_=tA[:, T - 1:T]), reads=["tA"], writes=["hcar"])
            P.op("dve", lambda e: e.tensor_tensor(out=tB[:], in0=G, in1=G, op=ALU.mult), reads=[kG], writes=["tB"])
            P.op("dve", lambda e: e.tensor_scalar(out=tB[:], in0=tB[:], scalar1=0.044715, scalar2=1.0, op0=ALU.mult, op1=ALU.add),
                 reads=["tB"], writes=["tB"])
            P.op("dve", lambda e: e.tensor_tensor(out=tB[:], in0=tB[:], in1=G, op=ALU.mult), reads=["tB", kG], writes=["tB"])
            P.op("act", lambda e: e.activation(out=tB[:], in_=tB[:], func=AF.Sigmoid, scale=1.5957691216057308),
                 reads=["tB"], writes=["tB"])
            P.op("dve", lambda e: e.tensor_tensor(out=tB[:], in0=tB[:], in1=G, op=ALU.mult), reads=["tB", kG], writes=["tB"])
            P.op("dve", lambda e: e.tensor_tensor(out=mixT[:, 6 + i, :], in0=tA[:], in1=tB[:], op=ALU.mult),
                 reads=["tA", "tB"], writes=["mixT"])

        def mixer(l, t):
            norm([(xres[:, s, :], xkh(s)) for s in range(NSUB)], 3 * l + 0, "T")
            P.op("dve", lambda e: e.tensor_copy(out=kdup[:, :, :, 0:128], in_=kcar[:, l, :, :].rearrange("p (a b) n -> p a b n", a=2)), reads=["kcar"], writes=["kdup"])
            P.op("dve", lambda e: e.tensor_copy(out=vpad[:, 0, :, :, :], in_=vcar[:, l, :, :].rearrange("p (a b) n -> p a b n", a=2)),
                 reads=["vcar", "vpad_all"], writes=["vp0"])
            brpos = [0]
            brmap = {}

            def brnext(name):
                j = brpos[0] % NBR
                brpos[0] += 1
                brmap[name] = j
                return br[j][:], f"br{j}"

            for pc in range(5):
                (wv_,), wkey = acquire(f"w_in{l}_{pc}")
                if pc == 0:
                    for c in range(4):
                        proj_fm(wv_, wkey, c, T, "act" if c % 2 == 0 else "dve", qT[:, c, :], "qT")
                elif pc == 4:
                    for b in range(NSUB):
                        bi = bank("mm")
                        for kc in range(KC):
                            P.op("pe", lambda e, kc=kc: e.matmul(psb[bi][:, 0:128], lhsT=hT[:, kc, b * 128:(b + 1) * 128],
                                                                 rhs=wv_[:, kc, 0:128], start=(kc == 0), stop=(kc == KC - 1)),
                                 reads=[wkey, "hT"], writes=[pk(bi)])
                        for j in range(2):
                            for eo in range(2):
                                evac("act" if eo == 0 else "dve", vpad[:, b + 1, j, eo, eo * 64:eo * 64 + 64],
                                     psb[bi][:, j * 64:(j + 1) * 64], [pk(bi), "vpad_all"], [f"vp{b + 1}"])
                else:
                    names = {1: ["K0", "K1", "B0", "C0"], 2: ["X0", "B1", "C1", "X1"], 3: ["R0", "G0", "R1", "G1"]}[pc]
                    for c, nm in enumerate(names):
                        if nm[0] == "K":
                            j = int(nm[1])
                            bi = bank("mm")
                            for kc in range(KC):
                                P.op("pe", lambda e, kc=kc: e.matmul(psb[bi][:], lhsT=wv_[:, kc, c * 128:(c + 1) * 128],
                                                                     rhs=hT[:, kc, :], start=(kc == 0), stop=(kc == KC - 1)),
                                     reads=[wkey, "hT"], writes=[pk(bi)])
                            evac("act", kdup[0:64, j, 0, 128:128 + T], psb[bi][0:64, :], [pk(bi)], ["kdup"])
                            evac("dve", kdup[64:128, j, 1, 128:128 + T], psb[bi][64:128, :], [pk(bi)], ["kdup"])
                            continue
                        if nm[0] == "R":
                            proj_fm(wv_, wkey, c, T, "act", xbuf[:, 3:3 + T], "xbuf")
                            continue
                        ap_, key_ = brnext(nm)
                        proj_fm(wv_, wkey, c, T, "act" if c % 2 == 0 else "dve", ap_, key_)
                        if nm in ("X0", "X1") and DEBUG["sc"]:
                            i = int(nm[1])
                            jb, jc, jx = brmap[f"B{i}"], brmap[f"C{i}"], brmap[f"X{i}"]
                            sc_branch(l, i, f"br{jb}", f"br{jc}", f"br{jx}", br[jb][:], br[jc][:], br[jx][:])
                        if nm in ("G0", "G1") and DEBUG["rg"]:
                            i = int(nm[1])
                            jg = brmap[f"G{i}"]
                            rg_branch(l, i, None, f"br{jg}", None, br[jg][:])
            for b in range(NSUB if DEBUG["att"] else 0):
                gb = t * NSUB + b
                ba = [bank("att4") for _ in range(4)]
                for h in range(8):
                    c, eo, hg = h // 2, h % 2, h // 4
                    bi = ba[h // 2]
                    P.op("pe", lambda e, c=c, eo=eo, bi=bi, h=h, hg=hg: e.matmul(
                        psb[bi][:, (h % 2) * 256:(h % 2 + 1) * 256],
                        lhsT=qT[:, c, b * 128:(b + 1) * 128],
                        rhs=kdup[:, hg, eo, b * 128:b * 128 + 256], start=True, stop=True),
                         reads=["qT", "kdup"], writes=[pk(bi)])
                for pr in range(4):
                    P.op("dve", lambda e, pr=pr: e.scalar_tensor_tensor(
                        out=Lb[:, 2 * pr:2 * pr + 2, :], in0=psb[ba[pr]][:].rearrange("p (h k) -> p h k", h=2),
                        scalar=0.125, in1=bias8[:, 2 * pr:2 * pr + 2, :], op0=ALU.mult, op1=ALU.add),
                         reads=[pk(ba[pr]), "bias8"], writes=["Lb"])
                if gb == 0:
                    P.op("dve", lambda e: e.memset(Lb[:, :, 0:128], NEG), writes=["Lb"])
                softmax_block(8, sinks[:, 8 * l:8 * l + 8])
                bo = bank("mm")
                for c in range(4):
                    for eo in range(2):
                        h = 2 * c + eo
                        for kb in range(2):
                            P.op("pe", lambda e, c=c, eo=eo, h=h, kb=kb: e.matmul(
                                psb[bo][:, c * 128:(c + 1) * 128], lhsT=vpad[:, b + kb, c // 2, eo, :],
                                rhs=PT[:, h, kb, :], start=(eo == 0 and kb == 0), stop=(eo == 1 and kb == 1)),
                                 reads=["PT", f"vp{b + kb}", "vpad_all"], writes=[pk(bo)])
                evac("dve", mixT[:, 0:4, b * 128:(b + 1) * 128],
                     psb[bo][:].rearrange("p (c n) -> p c n", c=4), [pk(bo)], ["mixT"])
            P.op("dve", lambda e: e.tensor_copy(out=kcar[:, l, :, :].rearrange("p (a b) n -> p a b n", a=2), in_=kdup[:, :, :, T:T + 128]), reads=["kdup"], writes=["kcar"])
            P.op("dve", lambda e: e.tensor_copy(out=vcar[:, l, :, :].rearrange("p (a b) n -> p a b n", a=2), in_=vpad[:, 4, :, :, :]),
                 reads=["vp4"], writes=["vcar"])
            for hf in range(2):
                (wv_,), wkey = acquire(f"w_out{l}_{hf}")
                for s in range(NSUB):
                    bi = bank("mm")
                    for kc in range(KC):
                        P.op("pe", lambda e, kc=kc: e.matmul(psb[bi][:], lhsT=mixT[:, kc, s * 128:(s + 1) * 128],
                                                             rhs=wv_[:, kc, :], start=(kc == 0), stop=(kc == KC - 1)),
                             reads=[wkey, "mixT"], writes=[pk(bi)])
                    resid_add(s, hf, bi)

        def xattn(l, t):
            norm([(xres[:, s, :], xkh(s)) for s in range(NSUB)], 3 * l + 1, "T")
            (wv_,), wkey = acquire(f"xa_wq{l}")
            for c in range(4):
                proj_fm(wv_, wkey, c, T, "act" if c % 2 == 0 else "dve", qT[:, c, :], "qT")
            sc_ = 1.0 / math.sqrt(128.0)
            for b0 in range(0, NSUB, 2):
                ba = [bank("att4") for _ in range(4)]
                for v in range(8):
                    bb, h = v // 4, v % 4
                    b = b0 + bb
                    bi = ba[v // 2]
                    P.op("pe", lambda e, h=h, bi=bi, v=v, b=b: e.matmul(psb[bi][:, (v % 2) * 256:(v % 2 + 1) * 256],
                                                                     lhsT=qT[:, h, b * 128:(b + 1) * 128], rhs=kmT[:, l, h, :],
                                                                     start=True, stop=True),
                         reads=["qT", "kmT"], writes=[pk(bi)])
                for pr in range(4):
                    P.op("dve", lambda e, pr=pr: e.tensor_scalar(out=Lb[:, 2 * pr:2 * pr + 2, :],
                                                                 in0=psb[ba[pr]][:].rearrange("p (h k) -> p h k", h=2),
                                                                 scalar1=sc_, scalar2=None, op0=ALU.mult),
                         reads=[pk(ba[pr])], writes=["Lb"])
                softmax_block(8, None)
                for bb in range(2):
                    b = b0 + bb
                    bo = bank("mm")
                    for h in range(4):
                        v = bb * 4 + h
                        for kb in range(2):
                            P.op("pe", lambda e, h=h, kb=kb, v=v: e.matmul(psb[bo][:, h * 128:(h + 1) * 128],
                                                                          lhsT=vm[:, l, kb, h * 128:(h + 1) * 128],
                                                                          rhs=PT[:, v, kb, :], start=(kb == 0), stop=(kb == 1)),
                                 reads=["PT", "vm"], writes=[pk(bo)])
                    evac("dve" if bb == 0 else "act", mixT[:, 0:4, b * 128:(b + 1) * 128],
                         psb[bo][:].rearrange("p (c n) -> p c n", c=4), [pk(bo)], ["mixT"])
            (wo_,), wkey = acquire(f"xa_wo{l}")
            for hf in range(2):
                for s in range(NSUB):
                    bi = bank("mm")
                    for kc in range(4):
                        P.op("pe", lambda e, kc=kc: e.matmul(psb[bi][:], lhsT=mixT[:, kc, s * 128:(s + 1) * 128],
                                                             rhs=wo_[:, kc, hf * 512:(hf + 1) * 512],
                                                             start=(kc == 0), stop=(kc == 3)),
                             reads=[wkey, "mixT"], writes=[pk(bi)])
                    resid_add(s, hf, bi)

        fstate = dict(n=0)

        def ffn(l, t):
            norm([(xres[:, s, :], xkh(s)) for s in range(NSUB)], 3 * l + 2, "T", router=(l == 1))
            ne_, ng_ = (1, DFF // 256) if l == 0 else (nexp, DFE // 256)
            groups = [(e_, gi) for e_ in range(ne_) for gi in range(ng_)]

            def up(e_, gi):
                name = f"dense_{gi}" if l == 0 else f"moe{e_}_{gi}"
                (wg_, wu_, wd_), wkey = acquire(name)
                hj = fstate["n"] % 2
                fstate["n"] += 1
                hb, hk = h1[hj], f"h1{hj}"
                for cc in range(2):
                    bg, bu = bank("mm"), bank("mm")
                    for kc in range(KC):
                        P.op("pe", lambda e, kc=kc: e.matmul(psb[bg][:], lhsT=wg_[:, kc, cc * 128:(cc + 1) * 128],
                                                             rhs=hT[:, kc, :], start=(kc == 0), stop=(kc == KC - 1)),
                             reads=[wkey, "hT"], writes=[pk(bg)])
                    for kc in range(KC):
                        P.op("pe", lambda e, kc=kc: e.matmul(psb[bu][:], lhsT=wu_[:, kc, cc * 128:(cc + 1) * 128],
                                                             rhs=hT[:, kc, :], start=(kc == 0), stop=(kc == KC - 1)),
                             reads=[wkey, "hT"], writes=[pk(bu)])
                    sj = cc
                    P.op("act", lambda e: e.activation(out=sil[sj][:], in_=psb[bg][:], func=AF.Silu),
                         reads=[pk(bg)], writes=[f"sil{sj}"])
                    P.op("dve", lambda e: e.tensor_tensor(out=hb[:, cc, :], in0=sil[sj][:], in1=psb[bu][:], op=ALU.mult),
                         reads=[f"sil{sj}", pk(bu)], writes=[f"{hk}_{cc}"])
                return (hb, hk, wd_, wkey, e_)

            def down(stt):
                hb, hk, wd_, wkey, e_ = stt
                for s in range(NSUB):
                    for hf in range(2):
                        bi = bank("dn")
                        for cc in range(2):
                            P.op("pe", lambda e, cc=cc: e.matmul(psb[bi][:], lhsT=hb[:, cc, s * 128:(s + 1) * 128],
                                                                 rhs=wd_[:, cc, hf * 512:(hf + 1) * 512],
                                                                 start=(cc == 0), stop=(cc == 1)),
                                 reads=[wkey, f"{hk}_{cc}"], writes=[pk(bi)])
                        resid_add(s, hf, bi, None if l == 0 else gates[:, s, e_:e_ + 1])

            prev = None
            for gidx, (e_, gi) in enumerate(groups):
                cur = up(e_, gi)
                if t == 0 and l == 0 and cast_pending and cast_pending[0][0] == "E0":
                    cast_tick()
                if t == 0 and l == 1 and ((gidx + 1) * 4) // 7 > (gidx * 4) // 7:
                    cast_tick()
                if prev is not None:
                    down(prev)
                prev = cur
            down(prev)
            if t == 0 and l == 1:
                cast_tick(len(cast_pending))

        for t in range(NT):
            for s_ in range(NSUB):
                P.dma("sp", [(xres[:, s_, :], x_d[t * T + s_ * 128:t * T + (s_ + 1) * 128, :])], f"xld{s_}",
                      writes=xkh(s_))
            for l in range(2):
                st = l * 3
                if st < stop_after:
                    mixer(l, t)
                if t == 0 and l == 1:
                    cast_experts()
                if st + 1 < stop_after:
                    xattn(l, t)
                if st + 2 < stop_after:
                    ffn(l, t)
            norm([(xres[:, s, :], xkh(s)) for s in range(NSUB)], 6, "store", ybase=t * T)
        assert wstate["next"] == len(sched), (wstate, len(sched))
        for j in range(2):
            k = f"ost{j}"
            if k in P.sems:
                nc.sync.wait_ge(P.sems[k], P.cnt[k])
    return nc


def _t5_bucket(dist):
    n = np.maximum(dist, 0)
    large = 16 + (np.log(np.maximum(n, 1).astype(np.float32) / 16) / math.log(128 / 16) * 16).astype(np.int32)
    large = np.minimum(large, 31)
    return np.where(n < 16, n, large)


def prepare_shared(inp):
    f = lambda a: np.ascontiguousarray(np.asarray(a, dtype=np.float32))
    w_in = f(inp["w_in"])
    q, k, v = w_in[:, :, 0:512], w_in[:, :, 512:640], w_in[:, :, 640:768]
    Bc, Cc, Xc = w_in[:, :, 768:1024], w_in[:, :, 1024:1280], w_in[:, :, 1280:1536]
    Rc, Gc = w_in[:, :, 1536:1792], w_in[:, :, 1792:2048]
    cols = [q, k[:, :, 0:64], k[:, :, 0:64], k[:, :, 64:128], k[:, :, 64:128]]
    for i in range(2):
        cols += [Bc[:, :, i * 128:(i + 1) * 128], Cc[:, :, i * 128:(i + 1) * 128], Xc[:, :, i * 128:(i + 1) * 128]]
    for i in range(2):
        cols += [Rc[:, :, i * 128:(i + 1) * 128], Gc[:, :, i * 128:(i + 1) * 128]]
    cols += [v]
    w_inp = np.ascontiguousarray(np.concatenate(cols, axis=2))
    assert w_inp.shape[2] == INC
    gains = np.concatenate([
        np.stack([f(inp["mix_norm"])[0], f(inp["xa_norm"])[0], f(inp["ffn_norm"])[0],
                  f(inp["mix_norm"])[1], f(inp["xa_norm"])[1], f(inp["ffn_norm"])[1],
                  f(inp["final_norm"])], 0),
        f(inp["mem_norm"])], 0)
    qi = np.arange(128)[:, None]
    ki = np.arange(256)[None, :]
    dist = qi + 128 - ki
    bucket = _t5_bucket(dist)
    rel = f(inp["rel_bias"])
    biasg = np.ascontiguousarray(rel[bucket].transpose(0, 2, 1))
    maskc = np.where((dist >= 0) & (dist < 128), 0.0, NEG).astype(np.float32)
    chanp = np.zeros((128, 48), np.float32)
    for l in range(2):
        for i in range(2):
            sl = slice(i * 128, (i + 1) * 128)
            cb = 24 * l + i * 4
            chanp[:, cb:cb + 3] = f(inp["sc_conv_w"])[l][:, sl].T
            chanp[:, cb + 3] = f(inp["sc_conv_b"])[l][sl]
            cb = 24 * l + 8 + i * 5
            chanp[:, cb:cb + 4] = f(inp["rg_conv_w"])[l][:, sl].T
            chanp[:, cb + 4] = f(inp["rg_conv_b"])[l][sl]
            cg = 24 * l + 18 + i * 3
            chanp[:, cg] = f(inp["rg_b_a"])[l][sl]
            chanp[:, cg + 1] = f(inp["rg_b_x"])[l][sl]
            chanp[:, cg + 2] = f(inp["rg_lambda"])[l][sl]
    rgw = np.ascontiguousarray(np.stack([f(inp["rg_w_a"]), f(inp["rg_w_x"])], 1))
    shared = {
        "w_inp": w_inp, "w_out": f(inp["w_out"]), "xa_wq": f(inp["xa_wq"]), "xa_wk": f(inp["xa_wk"]),
        "xa_wv": f(inp["xa_wv"]), "xa_wo": f(inp["xa_wo"]),
        "dense_wg": f(inp["dense_wg"]), "dense_wu": f(inp["dense_wu"]), "dense_wd": f(inp["dense_wd"]),
        "moe_wg": f(inp["moe_wg"])[0], "moe_wu": f(inp["moe_wu"])[0], "moe_wd": f(inp["moe_wd"])[0],
        "router": f(inp["moe_router"])[0], "gains": np.ascontiguousarray(gains),
        "biasg": biasg, "maskc": maskc, "sinks": f(inp["attn_sinks"]).reshape(1, 16),
        "chanp": chanp, "rgw": rgw, "ident": np.eye(128, dtype=np.float32),
    }
    return shared


def kernel(**inputs):
    shared = prepare_shared(inputs)
    x = np.asarray(inputs["x"], dtype=np.float32)
    mem = np.asarray(inputs["mem"], dtype=np.float32)
    nc = bass.Bass("TRN2", target_bir_lowering=False)
    build_program(nc, SEQ // T)
    in_maps = []
    for c in range(NCORES):
        m = dict(shared)
        m["x"] = np.ascontiguousarray(x[c])
        m["mem"] = np.ascontiguousarray(mem[c])
        in_maps.append(m)
    res = run_bass_kernel_spmd(nc, in_maps, core_ids=list(range(NCORES)))
    return np.stack([np.asarray(r["y"], dtype=np.float32) for r in res.results], 0)
```

```python
import contextlib
import math
import numpy as np
import concourse.bass as bass
import concourse.mybir as mybir
from concourse.bass_utils import run_bass_kernel_spmd

F32 = mybir.dt.float32
BF16 = mybir.dt.bfloat16
ALU = mybir.AluOpType
AF = mybir.ActivationFunctionType
AX = mybir.AxisListType

D = 1024
KC = 8
T = 512
NSUB = 4
SEQ = 4096
NCORES = 8
INC = 2176
DFF = 2816
DFE = 3584
NE = 8
SLOT = 6144
NSLOT = 4
NEG = -1.0e30
SELF_SYNC = True
SELF_WAR = False
DEBUG = {"att": True, "sc": True, "rg": True, "wout": True}


class Prog:
    def __init__(self, nc, es):
        self.nc = nc
        self.es = es
        self.engs = {"pe": nc.tensor, "act": nc.scalar, "dve": nc.vector, "pool": nc.gpsimd, "sp": nc.sync}
        self.sems = {}
        self.cnt = {}
        for n in self.engs:
            self.sems[n] = es.enter_context(nc.semaphore("sem_" + n))
            self.cnt[n] = 0
        self.waited = {n: {} for n in self.engs}
        self.lastw = {}
        self.readers = {}

    def new_sem(self, key):
        if key not in self.sems:
            self.sems[key] = self.es.enter_context(self.nc.semaphore("sem_" + key.replace(":", "_")))
            self.cnt[key] = 0

    def _deps(self, reads, writes, extra=()):
        deps = {}

        def add(d):
            if d is not None:
                deps[d[0]] = max(deps.get(d[0], 0), d[1])

        for k in reads:
            add(self.lastw.get(k))
        for k in writes:
            add(self.lastw.get(k))
        self._raw_self = dict(deps)
        for k in writes:
            for sk, v in self.readers.get(k, {}).items():
                add((sk, v))
        for d in extra:
            add(d)
        return deps

    def _wait(self, x, deps):
        for sk, v in deps.items():
            if sk == x and (x == "pe" or not SELF_SYNC):
                continue
            if sk == x and not SELF_WAR:
                v = self._raw_self.get(x, 0)
                if v == 0:
                    continue
            if self.waited[x].get(sk, 0) >= v:
                continue
            self.engs[x].wait_ge(self.sems[sk], v)
            self.waited[x][sk] = v

    def op(self, x, fn, reads=(), writes=()):
        self._wait(x, self._deps(reads, writes))
        ins = fn(self.engs[x])
        self.cnt[x] += 1
        ins.then_inc(self.sems[x], 1)
        c = self.cnt[x]
        for k in reads:
            self.readers.setdefault(k, {})[x] = c
        for k in writes:
            self.lastw[k] = (x, c)
            self.readers[k] = {}

    def dma(self, q, pairs, semkey, reads=(), writes=(), extra=()):
        self.new_sem(semkey)
        self._wait(q, self._deps(reads, writes, extra))
        for out, in_ in pairs:
            ins = self.engs[q].dma_start(out=out, in_=in_)
            ins.then_inc(self.sems[semkey], 16)
            self.cnt[semkey] += 16
        c = self.cnt[semkey]
        for k in reads:
            self.readers.setdefault(k, {})[semkey] = c
        for k in writes:
            self.lastw[k] = (semkey, c)
            self.readers[k] = {}


def build_program(nc, NT, stop_after=99, nexp=NE):
    S = NT * T
    es = contextlib.ExitStack()
    with es:
        P = Prog(nc, es)

        def din(name, shape, dt=F32):
            return nc.dram_tensor(name, list(shape), dt, kind="ExternalInput").ap()

        def dint(name, shape, dt=BF16):
            return nc.dram_tensor(name, list(shape), dt, kind="Internal").ap()

        x_d = din("x", [S, D])
        mem_d = din("mem", [256, D])
        y_d = nc.dram_tensor("y", [S, D], F32, kind="ExternalOutput").ap()
        w = {}
        wshapes = {
            "w_inp": [2, D, INC], "w_out": [2, D, D], "xa_wq": [2, D, 512], "xa_wk": [2, D, 512],
            "xa_wv": [2, D, 512], "xa_wo": [2, 512, D], "dense_wg": [1, D, DFF], "dense_wu": [1, D, DFF],
            "dense_wd": [1, DFF, D], "moe_wg": [NE, D, DFE], "moe_wu": [NE, D, DFE], "moe_wd": [NE, DFE, D],
        }
        w16 = {}
        for k, shp in wshapes.items():
            w[k] = din(k, shp)
            w16[k] = dint(k + "_16", shp)
        router_d = din("router", [D, NE])
        gains_d = din("gains", [9, D])
        biasg_d = din("biasg", [128, 8, 256])
        maskc_d = din("maskc", [128, 256])
        sinks_d = din("sinks", [1, 16])
        chanp_d = din("chanp", [128, 48])
        rgw_d = din("rgw", [2, 2, 4, 64, 64])
        ident_d = din("ident", [128, 128])

        def sb(name, shape, dt=F32):
            return es.enter_context(nc.sbuf_tensor(name, list(shape), dt))

        xres = sb("xres", [128, NSUB, D])
        bias8 = sb("bias8", [128, 8, 256])
        kdup = sb("kdup", [128, 2, 2, 128 + T], BF16)
        vpad = sb("vpad", [128, 5, 2, 2, 128], BF16)
        kcar = sb("kcar", [128, 2, 4, 128], BF16)
        vcar = sb("vcar", [128, 2, 4, 128], BF16)
        ucar = sb("ucar", [128, 2, 2, 2])
        xcar = sb("xcar", [128, 2, 2, 3])
        hcar = sb("hcar", [128, 2, 2, 1])
        kmT = sb("kmT", [128, 2, 4, 256], BF16)
        vm = sb("vm", [128, 2, 2, 512], BF16)
        ident32 = sb("ident32", [128, 128])
        identb = sb("identb", [128, 128], BF16)
        chanp = sb("chanp_sb", [128, 48])
        dcp = sb("dcp", [128, 2, 2, 2])
        sinks = sb("sinks_sb", [128, 16])
        BD = sb("BD", [128, 2, 2, 2, 128], BF16)
        router_sb = sb("router_sb", [128, KC, NE])
        maskc = sb("maskc_sb", [128, 256])
        small = sb("small", [128, 128])
        xn = [sb("xn0", [128, D]), sb("xn1", [128, D])]
        gbc = [sb("gbc0", [128, D]), sb("gbc1", [128, D])]
        hT = sb("hT", [128, KC, T], BF16)
        hT32 = sb("hT32", [128, KC, 128])
        qT = sb("qT", [128, 4, T], BF16)
        NBR = 5
        br = [sb(f"br{i}", [128, T]) for i in range(NBR)]
        ubuf = sb("ubuf", [128, 2 + T])
        xbuf = sb("xbuf", [128, 3 + T])
        tA = sb("tA", [128, T])
        tB = sb("tB", [128, T])
        tC = sb("tC", [128, T])
        tD = sb("tD", [128, T])
        rg16 = sb("rg16", [128, T], BF16)
        Lb = sb("Lb", [128, 8, 256])
        Pn = sb("Pn", [128, 8, 256], BF16)
        PT = sb("PT", [128, 8, 2, 128], BF16)
        mixT = sb("mixT", [128, KC, T], BF16)
        h1 = [sb("h1a", [128, 2, T], BF16), sb("h1b", [128, 2, T], BF16)]
        sil = [sb("sil0", [128, T]), sb("sil1", [128, T])]
        gates = sb("gates", [128, NSUB, NE])
        slots = [sb(f"slot{i}", [128, SLOT], BF16) for i in range(NSLOT)]
        psb = [es.enter_context(nc.psum_tensor(f"ps{i}", [128, 512], F32)) for i in range(8)]

        _sc = [0]

        def sm(n, key):
            a = small[:, _sc[0]:_sc[0] + n]
            _sc[0] += n
            assert _sc[0] <= 128
            return a

        epsc = sm(1, "c")
        onec = sm(1, "c")
        ss = sm(1, "ss")
        rs = sm(1, "rs")
        rstd = sm(1, "rstd")
        mx = sm(8, "mx")
        negm = sm(8, "negm")
        rsum = sm(8, "rsum")
        dsk = sm(8, "dsk")
        rec = sm(8, "rec")
        ss4 = sm(4, "ss4")
        rs4 = sm(4, "rs4")
        rstd4 = sm(4, "rstd4")
        lg = sm(8, "lg")
        top8 = sm(8, "top8")
        nm1 = sm(1, "nm1")
        sel = sm(8, "sel")
        ex = sm(8, "ex")
        gsm = sm(1, "gsm")
        grc = sm(1, "grc")
        lam_t = sm(8, "lam")

        rot = {"mm": [0, 1, 2, 3], "tr": [4, 5], "att": [6, 7], "dn": [4, 5, 6, 7], "att4": [4, 5, 6, 7]}
        rpos = {"mm": 0, "tr": 0, "att": 0, "dn": 0, "att4": 0}

        def bank(role):
            i = rot[role][rpos[role] % len(rot[role])]
            rpos[role] += 1
            return i

        def pk(i):
            return f"ps{i}"

        P.new_sem("const")
        cpairs = [
            (ident32[:], ident_d), (chanp[:], chanp_d), (maskc[:], maskc_d), (bias8[:], biasg_d),
            (sinks[:], sinks_d[0, :].partition_broadcast(128)),
            (router_sb[:], router_d.rearrange("(k p) e -> p k e", p=128)),
        ]
        ckeys = ["ident32", "chanp", "maskc", "bias8", "sinks", "router"]
        P.dma("sp", cpairs, "const", writes=ckeys)

        def pset(ap, val, key):
            P.op("pool", lambda e: e.memset(ap, val), writes=[key])

        pset(BD[:], 0.0, "BD")
        pset(vpad[:], 0.0, "vpad_all")
        pset(kdup[:], 0.0, "kdup")
        pset(kcar[:], 0.0, "kcar")
        pset(vcar[:], 0.0, "vcar")
        pset(ucar[:], 0.0, "ucar")
        pset(xcar[:], 0.0, "xcar")
        pset(hcar[:], 0.0, "hcar")
        pset(epsc, 1e-6, "c")
        pset(onec, 1.0, "c")
        bdpairs = []
        for l in range(2):
            for ax in range(2):
                for hh in range(4):
                    i, o = hh // 2, (hh % 2) * 64
                    bdpairs.append((BD[o:o + 64, l, ax, i, o:o + 64], rgw_d[l, ax, hh]))
        P.dma("pool", bdpairs, "bdld", writes=["BD"])

        cast_total = {}

        cast_tot = {}
        cast_pending = []
        cast_issued = []

        def cast_issue(item):
            grp, d_, s_ = item
            nc.gpsimd.dma_start(out=d_, in_=s_).then_inc(P.sems["cast:" + grp], 16)
            P.cnt["cast:" + grp] += 16
            cast_issued.append(("cast:" + grp, P.cnt["cast:" + grp]))

        def cast_tick(n=1):
            for _ in range(n):
                if not cast_pending:
                    return
                nc.gpsimd.wait_ge(P.sems["pe"], P.cnt["pe"])
                if len(cast_issued) >= 2:
                    sk, v = cast_issued[-2]
                    nc.gpsimd.wait_ge(P.sems[sk], v)
                cast_issue(cast_pending.pop(0))

        def cast2d(src, dst, grp, defer=False):
            P.new_sem("cast:" + grp)
            R, C = src.shape
            ns = (C + 2047) // 2048
            assert C % ns == 0
            RB = 1024 if C <= 2048 else 512
            for r0 in range(0, R, RB):
                r1 = min(R, r0 + RB)
                s_ = src[r0:r1, :].rearrange("r (a c) -> r a c", a=ns)
                d_ = dst[r0:r1, :].rearrange("r (a c) -> r a c", a=ns)
                cast_tot[grp] = cast_tot.get(grp, 0) + 16
                if defer:
                    cast_pending.append((grp, d_, s_))
                else:
                    cast_issue((grp, d_, s_))

        def cast_fence(grp):
            nc.gpsimd.wait_ge(P.sems["cast:" + grp], P.cnt["cast:" + grp])

        for l in range(2):
            cast2d(w["xa_wk"][l], w16["xa_wk"][l], "P")
            cast2d(w["xa_wv"][l], w16["xa_wv"][l], "P")
        cast_fence("P")
        for k in ["w_inp", "w_out", "xa_wq", "xa_wo"]:
            cast2d(w[k][0], w16[k][0], "A")
        cast_fence("A")
        for k in ["dense_wg", "dense_wu", "dense_wd"]:
            cast2d(w[k][0], w16[k][0], "B")
        for k in ["w_inp", "w_out", "xa_wq", "xa_wo"]:
            cast2d(w[k][1], w16[k][1], "C")
        cast_fence("B")
        for e in range(nexp):
            P.new_sem(f"cast:E{e}")

        def cast_experts():
            cast_tick(len([1 for it in cast_pending if it[0] == "E0"]))

        for e in range(nexp):
            for k in ["moe_wg", "moe_wu", "moe_wd"]:
                cast2d(w[k][e], w16[k][e], f"E{e}", defer=True)

        def castdep(grp):
            return ("cast:" + grp, cast_tot[grp])

        def kview(slot, off, k, n):
            return slot[:, off:off + k * n].rearrange("p (k n) -> p k n", k=k)

        def piece_cols(name, src2d, c0, c1, grp, k=KC):
            n = c1 - c0
            return dict(name=name, grp=grp,
                        parts=[(lambda s, k=k, n=n: kview(s, 0, k, n),
                                src2d[:, c0:c1].rearrange("(k p) n -> p k n", p=128))])

        def piece_ffn(name, wg, wu, wd, gi, grp):
            f0 = gi * 256
            return dict(name=name, grp=grp, parts=[
                (lambda s: kview(s, 0, KC, 256), wg[:, f0:f0 + 256].rearrange("(k p) n -> p k n", p=128)),
                (lambda s: kview(s, 2048, KC, 256), wu[:, f0:f0 + 256].rearrange("(k p) n -> p k n", p=128)),
                (lambda s: kview(s, 4096, 2, D), wd[f0:f0 + 256, :].rearrange("(c p) n -> p c n", p=128)),
            ])

        sched = []
        for l in range(2):
            sched.append(piece_cols(f"xa_wk{l}", w16["xa_wk"][l], 0, 512, "P"))
            sched.append(piece_cols(f"xa_wv{l}", w16["xa_wv"][l], 0, 512, "P"))
        NSTAGE = 0
        for t in range(NT):
            for l in range(2):
                g = "A" if l == 0 else "C"
                st = l * 3
                if st < stop_after:
                    for pc in range(5):
                        c0, c1 = pc * 512, min(INC, pc * 512 + 512)
                        sched.append(piece_cols(f"w_in{l}_{pc}", w16["w_inp"][l], c0, c1, g))
                    for hf in range(2):
                        sched.append(piece_cols(f"w_out{l}_{hf}", w16["w_out"][l], hf * 512, hf * 512 + 512, g))
                if st + 1 < stop_after:
                    sched.append(piece_cols(f"xa_wq{l}", w16["xa_wq"][l], 0, 512, g))
                    sched.append(piece_cols(f"xa_wo{l}", w16["xa_wo"][l], 0, D, g, k=4))
                if st + 2 < stop_after:
                    if l == 0:
                        for gi in range(DFF // 256):
                            sched.append(piece_ffn(f"dense_{gi}", w16["dense_wg"][0], w16["dense_wu"][0],
                                                   w16["dense_wd"][0], gi, "B"))
                    else:
                        for e in range(nexp):
                            for gi in range(DFE // 256):
                                sched.append(piece_ffn(f"moe{e}_{gi}", w16["moe_wg"][e], w16["moe_wu"][e],
                                                       w16["moe_wd"][e], gi, f"E{e}"))
        wstate = dict(issued=0, next=0)

        def acquire(name):
            i = wstate["next"]
            assert sched[i]["name"] == name, (sched[i]["name"], name)
            while wstate["issued"] < min(len(sched), i + NSLOT - 1):
                j = wstate["issued"]
                pcs = sched[j]
                sl = slots[j % NSLOT]
                P.dma("sp", [(mk(sl), src) for mk, src in pcs["parts"]], f"slot{j % NSLOT}",
                      writes=[f"slot{j % NSLOT}"], extra=[castdep(pcs["grp"])])
                wstate["issued"] += 1
            wstate["next"] += 1
            sl = slots[i % NSLOT]
            return [mk(sl) for mk, _ in sched[i]["parts"]], f"slot{i % NSLOT}"

        for l in range(2):
            for i in range(2):
                col = 24 * l + 18 + 3 * i + 2
                j = (l * 2 + i) * 2
                P.op("act", lambda e, col=col, j=j: e.activation(out=lam_t[:, j:j + 1], in_=chanp[:, col:col + 1],
                                                                 func=AF.Exp, scale=-1.0),
                     reads=["chanp"], writes=["lam"])
                P.op("act", lambda e, j=j: e.activation(out=lam_t[:, j + 1:j + 2], in_=lam_t[:, j:j + 1],
                                                        func=AF.Ln, bias=onec, scale=1.0),
                     reads=["lam", "c"], writes=["lam"])
                P.op("dve", lambda e, l=l, i=i, j=j: e.tensor_scalar(out=dcp[:, l, i, 0:1], in0=lam_t[:, j + 1:j + 2],
                                                                     scalar1=-8.0, scalar2=None, op0=ALU.mult),
                     reads=["lam"], writes=["dcp"])
                P.op("dve", lambda e, l=l, i=i, j=j: e.tensor_scalar(out=dcp[:, l, i, 1:2], in0=lam_t[:, j + 1:j + 2],
                                                                     scalar1=-16.0, scalar2=None, op0=ALU.mult),
                     reads=["lam"], writes=["dcp"])
        for h in range(8):
            P.op("dve", lambda e, h=h: e.tensor_tensor(out=bias8[:, h, :], in0=bias8[:, h, :], in1=maskc[:], op=ALU.add),
                 reads=["bias8", "maskc"], writes=["bias8"])
        P.op("dve", lambda e: e.tensor_copy(out=identb[:], in_=ident32[:]), reads=["ident32"], writes=["identb"])

        gstate = dict(g=0, n=0)

        def load_gain(row):
            j = gstate["g"] % 2
            gstate["g"] += 1
            P.dma("sp", [(gbc[j][:], gains_d[row, :].partition_broadcast(128))], f"gbc{j}", writes=[f"gbc{j}"])
            return j

        def evac(eng, out, in_, reads, writes):
            if eng == "act":
                P.op("act", lambda e: e.activation(out=out, in_=in_, func=AF.Copy), reads=reads, writes=writes)
            else:
                P.op("dve", lambda e: e.tensor_copy(out=out, in_=in_), reads=reads, writes=writes)

        def norm(srcs, row, mode, col0=0, router=False, ybase=None):
            gj = load_gain(row)
            ns_ = len(srcs)
            junk = Lb[:].rearrange("p h k -> p (h k)")
            for si, (xa, xkey) in enumerate(srcs):
                xkeys = list(xkey) if isinstance(xkey, (list, tuple)) else [xkey]
                P.op("act", lambda e: e.activation(out=junk[:, (si % 2) * D:(si % 2 + 1) * D], in_=xa, func=AF.Square,
                                                   accum_out=ss4[:, si:si + 1]),
                     reads=xkeys, writes=["Lb", "ss4"])
            P.op("act", lambda e: e.activation(out=rs4[:, 0:ns_], in_=ss4[:, 0:ns_], func=AF.Sqrt, bias=epsc, scale=1.0 / D),
                 reads=["ss4", "c"], writes=["rs4"])
            P.op("dve", lambda e: e.reciprocal(out=rstd4[:, 0:ns_], in_=rs4[:, 0:ns_]), reads=["rs4"], writes=["rstd4"])
            for si, (xa, xkey) in enumerate(srcs):
                j = gstate["n"] % 2
                gstate["n"] += 1
                xb = xn[j]
                xk = f"xn{j}"
                xkeys = list(xkey) if isinstance(xkey, (list, tuple)) else [xkey]
                P.op("dve", lambda e: e.scalar_tensor_tensor(out=xb[:], in0=xa, scalar=rstd4[:, si:si + 1], in1=gbc[gj][:],
                                                             op0=ALU.mult, op1=ALU.mult),
                     reads=xkeys + ["rstd4", f"gbc{gj}"], writes=[xk])
                if mode == "store":
                    P.dma("act", [(y_d[ybase + si * 128: ybase + (si + 1) * 128, :], xb[:])], f"ost{j}", reads=[xk])
                    continue
                b0, b1 = bank("tr"), bank("tr")
                for kc in range(KC):
                    bi = b0 if kc < 4 else b1
                    P.op("pe", lambda e, kc=kc, bi=bi: e.transpose(out=psb[bi][:, (kc % 4) * 128:(kc % 4 + 1) * 128],
                                                                  in_=xb[:, kc * 128:(kc + 1) * 128], identity=ident32[:]),
                         reads=[xk, "ident32"], writes=[pk(bi)])
                c = col0 + si * 128
                evac("act", hT[:, 0:4, c:c + 128], psb[b0][:].rearrange("p (k n) -> p k n", k=4), [pk(b0)], ["hT"])
                evac("dve", hT[:, 4:8, c:c + 128], psb[b1][:].rearrange("p (k n) -> p k n", k=4), [pk(b1)], ["hT"])
                if router:
                    evac("act", hT32[:, 0:4, :], psb[b0][:].rearrange("p (k n) -> p k n", k=4), [pk(b0)], ["hT32"])
                    evac("dve", hT32[:, 4:8, :], psb[b1][:].rearrange("p (k n) -> p k n", k=4), [pk(b1)], ["hT32"])
                    bl = bank("mm")
                    for kc in range(KC):
                        P.op("pe", lambda e, kc=kc: e.matmul(psb[bl][:, 0:NE], lhsT=hT32[:, kc, :], rhs=router_sb[:, kc, :],
                                                             start=(kc == 0), stop=(kc == KC - 1)),
                             reads=["hT32", "router"], writes=[pk(bl)])
                    P.op("dve", lambda e: e.tensor_copy(out=lg, in_=psb[bl][:, 0:NE]), reads=[pk(bl)], writes=["lg"])
                    P.op("dve", lambda e: e.max(out=top8, in_=lg), reads=["lg"], writes=["top8"])
                    P.op("dve", lambda e: e.tensor_scalar(out=nm1, in0=top8[:, 0:1], scalar1=-1.0, scalar2=None,
                                                          op0=ALU.mult), reads=["top8"], writes=["nm1"])
                    P.op("dve", lambda e: e.tensor_scalar(out=sel, in0=lg, scalar1=top8[:, 1:2], scalar2=None,
                                                          op0=ALU.is_ge), reads=["lg", "top8"], writes=["sel"])
                    P.op("act", lambda e: e.activation(out=ex, in_=lg, func=AF.Exp, bias=nm1, scale=1.0),
                         reads=["lg", "nm1"], writes=["ex"])
                    P.op("dve", lambda e: e.tensor_tensor(out=ex, in0=ex, in1=sel, op=ALU.mult),
                         reads=["ex", "sel"], writes=["ex"])
                    P.op("dve", lambda e: e.tensor_reduce(out=gsm, in_=ex, axis=AX.X, op=ALU.add),
                         reads=["ex"], writes=["gsm"])
                    P.op("dve", lambda e: e.reciprocal(out=grc, in_=gsm), reads=["gsm"], writes=["grc"])
                    P.op("dve", lambda e, si=si: e.tensor_scalar(out=gates[:, si, :], in0=ex, scalar1=grc, scalar2=None,
                                                                 op0=ALU.mult), reads=["ex", "grc"], writes=["gates"])

        def proj_fm(wv_, wkey, cidx, n_tok, evac_eng, out_ap, out_key, kcn=KC, rhs_cols=None):
            bi = bank("mm")
            c0, c1 = rhs_cols if rhs_cols else (0, n_tok)
            for kc in range(kcn):
                P.op("pe", lambda e, kc=kc: e.matmul(psb[bi][:, 0:n_tok], lhsT=wv_[:, kc, cidx * 128:(cidx + 1) * 128],
                                                     rhs=hT[:, kc, c0:c1], start=(kc == 0), stop=(kc == kcn - 1)),
                     reads=[wkey, "hT"], writes=[pk(bi)])
            evac(evac_eng, out_ap, psb[bi][:, 0:n_tok], [pk(bi)], [out_key])

        def softmax_block(nh, sink_ap):
            P.op("dve", lambda e: e.tensor_reduce(out=mx[:, 0:nh], in_=Lb[:, 0:nh, :], axis=AX.X, op=ALU.max),
                 reads=["Lb"], writes=["mx"])
            if sink_ap is not None:
                P.op("dve", lambda e: e.tensor_tensor(out=mx[:, 0:nh], in0=mx[:, 0:nh], in1=sink_ap, op=ALU.max),
                     reads=["mx", "sinks"], writes=["mx"])
            P.op("dve", lambda e: e.tensor_scalar(out=negm[:, 0:nh], in0=mx[:, 0:nh], scalar1=-1.0, scalar2=None,
                                                  op0=ALU.mult), reads=["mx"], writes=["negm"])
            for hh in range(nh):
                P.op("act", lambda e, hh=hh: e.activation(out=Lb[:, hh, :], in_=Lb[:, hh, :], func=AF.Exp,
                                                          bias=negm[:, hh:hh + 1], scale=1.0,
                                                          accum_out=rsum[:, hh:hh + 1]),
                     reads=["Lb", "negm"], writes=["Lb", "rsum"])
            if sink_ap is not None:
                P.op("dve", lambda e: e.tensor_tensor(out=dsk[:, 0:nh], in0=sink_ap, in1=mx[:, 0:nh], op=ALU.subtract),
                     reads=["mx", "sinks"], writes=["dsk"])
                P.op("act", lambda e: e.activation(out=dsk[:, 0:nh], in_=dsk[:, 0:nh], func=AF.Exp),
                     reads=["dsk"], writes=["dsk"])
                P.op("dve", lambda e: e.tensor_tensor(out=rsum[:, 0:nh], in0=rsum[:, 0:nh], in1=dsk[:, 0:nh], op=ALU.add),
                     reads=["rsum", "dsk"], writes=["rsum"])
            P.op("dve", lambda e: e.reciprocal(out=rec[:, 0:nh], in_=rsum[:, 0:nh]), reads=["rsum"], writes=["rec"])
            for hh in range(nh):
                if hh % 2 == 0:
                    P.op("dve", lambda e, hh=hh: e.tensor_scalar(out=Pn[:, hh, :], in0=Lb[:, hh, :],
                                                                 scalar1=rec[:, hh:hh + 1], scalar2=None, op0=ALU.mult),
                         reads=["Lb", "rec"], writes=[f"Pn{hh}"])
                else:
                    P.op("act", lambda e, hh=hh: e.activation(out=Pn[:, hh, :], in_=Lb[:, hh, :], func=AF.Copy,
                                                              scale=rec[:, hh:hh + 1]),
                         reads=["Lb", "rec"], writes=[f"Pn{hh}"])
            nb_ = (nh + 3) // 4
            bts = [bank("mm") for _ in range(nb_)]
            for hh in range(nh):
                bt = bts[hh // 4]
                pv = psb[bt][:].bitcast(BF16)
                for kb in range(2):
                    o = ((hh % 4) * 2 + kb) * 128
                    P.op("pe", lambda e, hh=hh, kb=kb, o=o, pv=pv: e.transpose(out=pv[:, o:o + 128],
                                                                              in_=Pn[:, hh, kb * 128:(kb + 1) * 128],
                                                                              identity=identb[:]),
                         reads=[f"Pn{hh}", "identb"], writes=[pk(bt)])
            ptf = PT[:].rearrange("p h k n -> p (h k n)")
            for g_ in range(nb_):
                n_ = min(4, nh - 4 * g_) * 256
                evac("act" if g_ == 0 else "dve", ptf[:, g_ * 1024:g_ * 1024 + n_],
                     psb[bts[g_]][:].bitcast(BF16)[:, 0:n_], [pk(bts[g_])], ["PT"])

        P.dma("sp", [(xres[:, 0:2, :], mem_d.rearrange("(s p) d -> p s d", p=128))], "xld", writes=["x0", "x0b", "x1", "x1b"])
        for l in range(2):
            norm([(xres[:, 0, :], ["x0", "x0b"]), (xres[:, 1, :], ["x1", "x1b"])], 7 + l, "T")
            (wk_,), wkey = acquire(f"xa_wk{l}")
            for h in range(4):
                proj_fm(wk_, wkey, h, 256, "act" if h % 2 == 0 else "dve", kmT[:, l, h, :], "kmT")
            (wv_,), wkey = acquire(f"xa_wv{l}")
            for mt in range(2):
                bi = bank("mm")
                for kc in range(KC):
                    P.op("pe", lambda e, kc=kc: e.matmul(psb[bi][:], lhsT=hT[:, kc, mt * 128:(mt + 1) * 128],
                                                         rhs=wv_[:, kc, :], start=(kc == 0), stop=(kc == KC - 1)),
                         reads=[wkey, "hT"], writes=[pk(bi)])
                evac("act", vm[:, l, mt, :], psb[bi][:], [pk(bi)], ["vm"])

        xk_ = [f"x{s}" for s in range(NSUB)]
        xkh = lambda s: [f"x{s}", f"x{s}b"]
        xall = [k for s in range(NSUB) for k in xkh(s)]

        def resid_add(s, hf, bi, gate_ap=None):
            xa = xres[:, s, hf * 512:(hf + 1) * 512]
            if gate_ap is None:
                P.op("dve", lambda e: e.tensor_tensor(out=xa, in0=psb[bi][:], in1=xa, op=ALU.add),
                     reads=[pk(bi), xkh(s)[hf]], writes=[xkh(s)[hf]])
            else:
                P.op("dve", lambda e: e.scalar_tensor_tensor(out=xa, in0=psb[bi][:], scalar=gate_ap, in1=xa,
                                                             op0=ALU.mult, op1=ALU.add),
                     reads=[pk(bi), xkh(s)[hf], "gates"], writes=[xkh(s)[hf]])

        def sc_branch(l, i, kB, kC_, kX, B, C, X):
            cb = 24 * l + i * 4
            P.op("dve", lambda e: e.tensor_copy(out=ubuf[:, 0:2], in_=ucar[:, l, i, :]), reads=["ucar"], writes=["ubuf"])
            P.op("dve", lambda e: e.tensor_tensor(out=ubuf[:, 2:2 + T], in0=C, in1=X, op=ALU.mult),
                 reads=[kC_, kX], writes=["ubuf"])
            P.op("dve", lambda e: e.tensor_copy(out=ucar[:, l, i, :], in_=ubuf[:, T:T + 2]), reads=["ubuf"], writes=["ucar"])
            P.op("dve", lambda e: e.tensor_scalar(out=tA[:], in0=ubuf[:, 0:T], scalar1=chanp[:, cb:cb + 1],
                                                  scalar2=chanp[:, cb + 3:cb + 4], op0=ALU.mult, op1=ALU.add),
                 reads=["ubuf", "chanp"], writes=["tA"])
            for k in (1, 2):
                P.op("dve", lambda e, k=k: e.scalar_tensor_tensor(out=tA[:], in0=ubuf[:, k:k + T],
                                                                  scalar=chanp[:, cb + k:cb + k + 1], in1=tA[:],
                                                                  op0=ALU.mult, op1=ALU.add),
                     reads=["ubuf", "chanp", "tA"], writes=["tA"])
            P.op("dve", lambda e: e.tensor_tensor(out=mixT[:, 4 + i, :], in0=B, in1=tA[:], op=ALU.mult),
                 reads=[kB, "tA"], writes=["mixT"])

        def rg_branch(l, i, kR, kG, R, G):
            cb = 24 * l + 8 + i * 5
            cg = 24 * l + 18 + i * 3
            P.op("dve", lambda e: e.tensor_copy(out=xbuf[:, 0:3], in_=xcar[:, l, i, :]), reads=["xcar"], writes=["xbuf"])
            P.op("dve", lambda e: e.tensor_copy(out=xbuf[:, 3:3 + T], in_=R), reads=[kR], writes=["xbuf"])
            P.op("dve", lambda e: e.tensor_copy(out=xcar[:, l, i, :], in_=xbuf[:, T:T + 3]), reads=["xbuf"], writes=["xcar"])
            P.op("dve", lambda e: e.tensor_scalar(out=tA[:], in0=xbuf[:, 0:T], scalar1=chanp[:, cb:cb + 1],
                                                  scalar2=chanp[:, cb + 4:cb + 5], op0=ALU.mult, op1=ALU.add),
                 reads=["xbuf", "chanp"], writes=["tA"])
            for k in (1, 2, 3):
                P.op("dve", lambda e, k=k: e.scalar_tensor_tensor(out=tA[:], in0=xbuf[:, k:k + T],
                                                                  scalar=chanp[:, cb + k:cb + k + 1], in1=tA[:],
                                                                  op0=ALU.mult, op1=ALU.add),
                     reads=["xbuf", "chanp", "tA"], writes=["tA"])
            P.op("act", lambda e: e.activation(out=rg16[:], in_=tA[:], func=AF.Copy), reads=["tA"], writes=["rg16"])
            b_r, b_i = bank("mm"), bank("mm")
            P.op("pe", lambda e: e.matmul(psb[b_r][:], lhsT=BD[:, l, 0, i, :], rhs=rg16[:], start=True, stop=True),
                 reads=["BD", "rg16"], writes=[pk(b_r)])
            P.op("pe", lambda e: e.matmul(psb[b_i][:], lhsT=BD[:, l, 1, i, :], rhs=rg16[:], start=True, stop=True),
                 reads=["BD", "rg16"], writes=[pk(b_i)])
            P.op("act", lambda e: e.activation(out=tB[:], in_=psb[b_r][:], func=AF.Sigmoid, bias=chanp[:, cg:cg + 1], scale=1.0),
                 reads=[pk(b_r), "chanp"], writes=["tB"])
            P.op("act", lambda e: e.activation(out=tC[:], in_=psb[b_i][:], func=AF.Sigmoid, bias=chanp[:, cg + 1:cg + 2], scale=1.0),
                 reads=[pk(b_i), "chanp"], writes=["tC"])
            P.op("act", lambda e: e.activation(out=tD[:], in_=tB[:], func=AF.Exp, scale=dcp[:, l, i, 0:1]),
                 reads=["tB", "dcp"], writes=["tD"])
            P.op("act", lambda e: e.activation(out=tB[:], in_=tB[:], func=AF.Exp, scale=dcp[:, l, i, 1:2]),
                 reads=["tB", "dcp"], writes=["tB"])
            P.op("dve", lambda e: e.tensor_scalar(out=tB[:], in0=tB[:], scalar1=1.0, scalar2=-1.0, op0=ALU.min, op1=ALU.mult),
                 reads=["tB"], writes=["tB"])
            P.op("act", lambda e: e.activation(out=tB[:], in_=tB[:], func=AF.Sqrt, bias=onec, scale=1.0),
                 reads=["tB", "c"], writes=["tB"])
            P.op("dve", lambda e: e.tensor_tensor(out=tC[:], in0=tC[:], in1=tA[:], op=ALU.mult),
                 reads=["tC", "tA"], writes=["tC"])
            P.op("dve", lambda e: e.tensor_tensor(out=tC[:], in0=tC[:], in1=tB[:], op=ALU.mult),
                 reads=["tC", "tB"], writes=["tC"])
            P.op("dve", lambda e: e.tensor_tensor_scan(out=tA[:], data0=tD[:], data1=tC[:], initial=hcar[:, l, i, :],
                                                       op0=ALU.mult, op1=ALU.add),
                 reads=["tD", "tC", "hcar", "tA"], writes=["tA"])
            P.op("dve", lambda e: e.tensor_copy(out=hcar[:, l, i, :], in_=tA[:, T - 1:T]), reads=["tA"], writes=["hcar"])
            P.op("dve", lambda e: e.tensor_tensor(out=tB[:], in0=G, in1=G, op=ALU.mult), reads=[kG], writes=["tB"])
            P.op("dve", lambda e: e.tensor_scalar(out=tB[:], in0=tB[:], scalar1=0.044715, scalar2=1.0, op0=ALU.mult, op1=ALU.add),
                 reads=["tB"], writes=["tB"])
            P.op("dve", lambda e: e.tensor_tensor(out=tB[:], in0=tB[:], in1=G, op=ALU.mult), reads=["tB", kG], writes=["tB"])
            P.op("act", lambda e: e.activation(out=tB[:], in_=tB[:], func=AF.Sigmoid, scale=1.5957691216057308),
                 reads=["tB"], writes=["tB"])
            P.op("dve", lambda e: e.tensor_tensor(out=tB[:], in0=tB[:], in1=G, op=ALU.mult), reads=["tB", kG], writes=["tB"])
            P.op("dve", lambda e: e.tensor_tensor(out=mixT[:, 6 + i, :], in0=tA[:], in1=tB[:], op=ALU.mult),
                 reads=["tA", "tB"], writes=["mixT"])

        def mixer(l, t):
            norm([(xres[:, s, :], xkh(s)) for s in range(NSUB)], 3 * l + 0, "T")
            P.op("dve", lambda e: e.tensor_copy(out=kdup[:, :, :, 0:128], in_=kcar[:, l, :, :].rearrange("p (a b) n -> p a b n", a=2)), reads=["kcar"], writes=["kdup"])
            P.op("dve", lambda e: e.tensor_copy(out=vpad[:, 0, :, :, :], in_=vcar[:, l, :, :].rearrange("p (a b) n -> p a b n", a=2)),
                 reads=["vcar", "vpad_all"], writes=["vp0"])
            brpos = [0]
            brmap = {}

            def brnext(name):
                j = brpos[0] % NBR
                brpos[0] += 1
                brmap[name] = j
                return br[j][:], f"br{j}"

            for pc in range(5):
                (wv_,), wkey = acquire(f"w_in{l}_{pc}")
                if pc == 0:
                    for c in range(4):
                        proj_fm(wv_, wkey, c, T, "act" if c % 2 == 0 else "dve", qT[:, c, :], "qT")
                elif pc == 4:
                    for b in range(NSUB):
                        bi = bank("mm")
                        for kc in range(KC):
                            P.op("pe", lambda e, kc=kc: e.matmul(psb[bi][:, 0:128], lhsT=hT[:, kc, b * 128:(b + 1) * 128],
                                                                 rhs=wv_[:, kc, 0:128], start=(kc == 0), stop=(kc == KC - 1)),
                                 reads=[wkey, "hT"], writes=[pk(bi)])
                        for j in range(2):
                            for eo in range(2):
                                evac("act" if eo == 0 else "dve", vpad[:, b + 1, j, eo, eo * 64:eo * 64 + 64],
                                     psb[bi][:, j * 64:(j + 1) * 64], [pk(bi), "vpad_all"], [f"vp{b + 1}"])
                else:
                    names = {1: ["K0", "K1", "B0", "C0"], 2: ["X0", "B1", "C1", "X1"], 3: ["R0", "G0", "R1", "G1"]}[pc]
                    for c, nm in enumerate(names):
                        if nm[0] == "K":
                            j = int(nm[1])
                            bi = bank("mm")
                            for kc in range(KC):
                                P.op("pe", lambda e, kc=kc: e.matmul(psb[bi][:], lhsT=wv_[:, kc, c * 128:(c + 1) * 128],
                                                                     rhs=hT[:, kc, :], start=(kc == 0), stop=(kc == KC - 1)),
                                     reads=[wkey, "hT"], writes=[pk(bi)])
                            evac("act", kdup[0:64, j, 0, 128:128 + T], psb[bi][0:64, :], [pk(bi)], ["kdup"])
                            evac("dve", kdup[64:128, j, 1, 128:128 + T], psb[bi][64:128, :], [pk(bi)], ["kdup"])
                            continue
                        ap_, key_ = brnext(nm)
                        proj_fm(wv_, wkey, c, T, "act" if c % 2 == 0 else "dve", ap_, key_)
                        if nm in ("X0", "X1") and DEBUG["sc"]:
                            i = int(nm[1])
                            jb, jc, jx = brmap[f"B{i}"], brmap[f"C{i}"], brmap[f"X{i}"]
                            sc_branch(l, i, f"br{jb}", f"br{jc}", f"br{jx}", br[jb][:], br[jc][:], br[jx][:])
                        if nm in ("G0", "G1") and DEBUG["rg"]:
                            i = int(nm[1])
                            jr, jg = brmap[f"R{i}"], brmap[f"G{i}"]
                            rg_branch(l, i, f"br{jr}", f"br{jg}", br[jr][:], br[jg][:])
            for b in range(NSUB if DEBUG["att"] else 0):
                gb = t * NSUB + b
                ba = [bank("att4") for _ in range(4)]
                for h in range(8):
                    c, eo, hg = h // 2, h % 2, h // 4
                    bi = ba[h // 2]
                    P.op("pe", lambda e, c=c, eo=eo, bi=bi, h=h, hg=hg: e.matmul(
                        psb[bi][:, (h % 2) * 256:(h % 2 + 1) * 256],
                        lhsT=qT[:, c, b * 128:(b + 1) * 128],
                        rhs=kdup[:, hg, eo, b * 128:b * 128 + 256], start=True, stop=True),
                         reads=["qT", "kdup"], writes=[pk(bi)])
                for pr in range(4):
                    P.op("dve", lambda e, pr=pr: e.scalar_tensor_tensor(
                        out=Lb[:, 2 * pr:2 * pr + 2, :], in0=psb[ba[pr]][:].rearrange("p (h k) -> p h k", h=2),
                        scalar=0.125, in1=bias8[:, 2 * pr:2 * pr + 2, :], op0=ALU.mult, op1=ALU.add),
                         reads=[pk(ba[pr]), "bias8"], writes=["Lb"])
                if gb == 0:
                    P.op("dve", lambda e: e.memset(Lb[:, :, 0:128], NEG), writes=["Lb"])
                softmax_block(8, sinks[:, 8 * l:8 * l + 8])
                bo = bank("mm")
                for c in range(4):
                    for eo in range(2):
                        h = 2 * c + eo
                        for kb in range(2):
                            P.op("pe", lambda e, c=c, eo=eo, h=h, kb=kb: e.matmul(
                                psb[bo][:, c * 128:(c + 1) * 128], lhsT=vpad[:, b + kb, c // 2, eo, :],
                                rhs=PT[:, h, kb, :], start=(eo == 0 and kb == 0), stop=(eo == 1 and kb == 1)),
                                 reads=["PT", f"vp{b + kb}", "vpad_all"], writes=[pk(bo)])
                evac("dve", mixT[:, 0:4, b * 128:(b + 1) * 128],
                     psb[bo][:].rearrange("p (c n) -> p c n", c=4), [pk(bo)], ["mixT"])
            P.op("dve", lambda e: e.tensor_copy(out=kcar[:, l, :, :].rearrange("p (a b) n -> p a b n", a=2), in_=kdup[:, :, :, T:T + 128]), reads=["kdup"], writes=["kcar"])
            P.op("dve", lambda e: e.tensor_copy(out=vcar[:, l, :, :].rearrange("p (a b) n -> p a b n", a=2), in_=vpad[:, 4, :, :, :]),
                 reads=["vp4"], writes=["vcar"])
            for hf in range(2):
                (wv_,), wkey = acquire(f"w_out{l}_{hf}")
                for s in range(NSUB):
                    bi = bank("mm")
                    for kc in range(KC):
                        P.op("pe", lambda e, kc=kc: e.matmul(psb[bi][:], lhsT=mixT[:, kc, s * 128:(s + 1) * 128],
                                                             rhs=wv_[:, kc, :], start=(kc == 0), stop=(kc == KC - 1)),
                             reads=[wkey, "mixT"], writes=[pk(bi)])
                    resid_add(s, hf, bi)

        def xattn(l, t):
            norm([(xres[:, s, :], xkh(s)) for s in range(NSUB)], 3 * l + 1, "T")
            (wv_,), wkey = acquire(f"xa_wq{l}")
            for c in range(4):
                proj_fm(wv_, wkey, c, T, "act" if c % 2 == 0 else "dve", qT[:, c, :], "qT")
            sc_ = 1.0 / math.sqrt(128.0)
            for b0 in range(0, NSUB, 2):
                ba = [bank("att4") for _ in range(4)]
                for v in range(8):
                    bb, h = v // 4, v % 4
                    b = b0 + bb
                    bi = ba[v // 2]
                    P.op("pe", lambda e, h=h, bi=bi, v=v, b=b: e.matmul(psb[bi][:, (v % 2) * 256:(v % 2 + 1) * 256],
                                                                     lhsT=qT[:, h, b * 128:(b + 1) * 128], rhs=kmT[:, l, h, :],
                                                                     start=True, stop=True),
                         reads=["qT", "kmT"], writes=[pk(bi)])
                for pr in range(4):
                    P.op("dve", lambda e, pr=pr: e.tensor_scalar(out=Lb[:, 2 * pr:2 * pr + 2, :],
                                                                 in0=psb[ba[pr]][:].rearrange("p (h k) -> p h k", h=2),
                                                                 scalar1=sc_, scalar2=None, op0=ALU.mult),
                         reads=[pk(ba[pr])], writes=["Lb"])
                softmax_block(8, None)
                for bb in range(2):
                    b = b0 + bb
                    bo = bank("mm")
                    for h in range(4):
                        v = bb * 4 + h
                        for kb in range(2):
                            P.op("pe", lambda e, h=h, kb=kb, v=v: e.matmul(psb[bo][:, h * 128:(h + 1) * 128],
                                                                          lhsT=vm[:, l, kb, h * 128:(h + 1) * 128],
                                                                          rhs=PT[:, v, kb, :], start=(kb == 0), stop=(kb == 1)),
                                 reads=["PT", "vm"], writes=[pk(bo)])
                    evac("dve" if bb == 0 else "act", mixT[:, 0:4, b * 128:(b + 1) * 128],
                         psb[bo][:].rearrange("p (c n) -> p c n", c=4), [pk(bo)], ["mixT"])
            (wo_,), wkey = acquire(f"xa_wo{l}")
            for hf in range(2):
                for s in range(NSUB):
                    bi = bank("mm")
                    for kc in range(4):
                        P.op("pe", lambda e, kc=kc: e.matmul(psb[bi][:], lhsT=mixT[:, kc, s * 128:(s + 1) * 128],
                                                             rhs=wo_[:, kc, hf * 512:(hf + 1) * 512],
                                                             start=(kc == 0), stop=(kc == 3)),
                             reads=[wkey, "mixT"], writes=[pk(bi)])
                    resid_add(s, hf, bi)

        fstate = dict(n=0)

        def ffn(l, t):
            norm([(xres[:, s, :], xkh(s)) for s in range(NSUB)], 3 * l + 2, "T", router=(l == 1))
            ne_, ng_ = (1, DFF // 256) if l == 0 else (nexp, DFE // 256)
            groups = [(e_, gi) for e_ in range(ne_) for gi in range(ng_)]

            def up(e_, gi):
                name = f"dense_{gi}" if l == 0 else f"moe{e_}_{gi}"
                (wg_, wu_, wd_), wkey = acquire(name)
                hj = fstate["n"] % 2
                fstate["n"] += 1
                hb, hk = h1[hj], f"h1{hj}"
                for cc in range(2):
                    bg, bu = bank("mm"), bank("mm")
                    for kc in range(KC):
                        P.op("pe", lambda e, kc=kc: e.matmul(psb[bg][:], lhsT=wg_[:, kc, cc * 128:(cc + 1) * 128],
                                                             rhs=hT[:, kc, :], start=(kc == 0), stop=(kc == KC - 1)),
                             reads=[wkey, "hT"], writes=[pk(bg)])
                    for kc in range(KC):
                        P.op("pe", lambda e, kc=kc: e.matmul(psb[bu][:], lhsT=wu_[:, kc, cc * 128:(cc + 1) * 128],
                                                             rhs=hT[:, kc, :], start=(kc == 0), stop=(kc == KC - 1)),
                             reads=[wkey, "hT"], writes=[pk(bu)])
                    sj = cc
                    P.op("act", lambda e: e.activation(out=sil[sj][:], in_=psb[bg][:], func=AF.Silu),
                         reads=[pk(bg)], writes=[f"sil{sj}"])
                    P.op("dve", lambda e: e.tensor_tensor(out=hb[:, cc, :], in0=sil[sj][:], in1=psb[bu][:], op=ALU.mult),
                         reads=[f"sil{sj}", pk(bu)], writes=[f"{hk}_{cc}"])
                return (hb, hk, wd_, wkey, e_)

            def down(stt):
                hb, hk, wd_, wkey, e_ = stt
                for s in range(NSUB):
                    for hf in range(2):
                        bi = bank("dn")
                        for cc in range(2):
                            P.op("pe", lambda e, cc=cc: e.matmul(psb[bi][:], lhsT=hb[:, cc, s * 128:(s + 1) * 128],
                                                                 rhs=wd_[:, cc, hf * 512:(hf + 1) * 512],
                                                                 start=(cc == 0), stop=(cc == 1)),
                                 reads=[wkey, f"{hk}_{cc}"], writes=[pk(bi)])
                        resid_add(s, hf, bi, None if l == 0 else gates[:, s, e_:e_ + 1])

            prev = None
            for gidx, (e_, gi) in enumerate(groups):
                cur = up(e_, gi)
                if t == 0 and l == 0 and cast_pending and cast_pending[0][0] == "E0":
                    cast_tick()
                if t == 0 and l == 1 and ((gidx + 1) * 4) // 7 > (gidx * 4) // 7:
                    cast_tick()
                if prev is not None:
                    down(prev)
                prev = cur
            down(prev)
            if t == 0 and l == 1:
                cast_tick(len(cast_pending))

        for t in range(NT):
            for s_ in range(NSUB):
                P.dma("sp", [(xres[:, s_, :], x_d[t * T + s_ * 128:t * T + (s_ + 1) * 128, :])], f"xld{s_}",
                      writes=xkh(s_))
            for l in range(2):
                st = l * 3
                if st < stop_after:
                    mixer(l, t)
                if t == 0 and l == 1:
                    cast_experts()
                if st + 1 < stop_after:
                    xattn(l, t)
                if st + 2 < stop_after:
                    ffn(l, t)
            norm([(xres[:, s, :], xkh(s)) for s in range(NSUB)], 6, "store", ybase=t * T)
        assert wstate["next"] == len(sched), (wstate, len(sched))
        for j in range(2):
            k = f"ost{j}"
            if k in P.sems:
                nc.sync.wait_ge(P.sems[k], P.cnt[k])
    return nc


def _t5_bucket(dist):
    n = np.maximum(dist, 0)
    large = 16 + (np.log(np.maximum(n, 1).astype(np.float32) / 16) / math.log(128 / 16) * 16).astype(np.int32)
    large = np.minimum(large, 31)
    return np.where(n < 16, n, large)


def prepare_shared(inp):
    f = lambda a: np.ascontiguousarray(np.asarray(a, dtype=np.float32))
    w_in = f(inp["w_in"])
    q, k, v = w_in[:, :, 0:512], w_in[:, :, 512:640], w_in[:, :, 640:768]
    Bc, Cc, Xc = w_in[:, :, 768:1024], w_in[:, :, 1024:1280], w_in[:, :, 1280:1536]
    Rc, Gc = w_in[:, :, 1536:1792], w_in[:, :, 1792:2048]
    cols = [q, k[:, :, 0:64], k[:, :, 0:64], k[:, :, 64:128], k[:, :, 64:128]]
    for i in range(2):
        cols += [Bc[:, :, i * 128:(i + 1) * 128], Cc[:, :, i * 128:(i + 1) * 128], Xc[:, :, i * 128:(i + 1) * 128]]
    for i in range(2):
        cols += [Rc[:, :, i * 128:(i + 1) * 128], Gc[:, :, i * 128:(i + 1) * 128]]
    cols += [v]
    w_inp = np.ascontiguousarray(np.concatenate(cols, axis=2))
    assert w_inp.shape[2] == INC
    gains = np.concatenate([
        np.stack([f(inp["mix_norm"])[0], f(inp["xa_norm"])[0], f(inp["ffn_norm"])[0],
                  f(inp["mix_norm"])[1], f(inp["xa_norm"])[1], f(inp["ffn_norm"])[1],
                  f(inp["final_norm"])], 0),
        f(inp["mem_norm"])], 0)
    qi = np.arange(128)[:, None]
    ki = np.arange(256)[None, :]
    dist = qi + 128 - ki
    bucket = _t5_bucket(dist)
    rel = f(inp["rel_bias"])
    biasg = np.ascontiguousarray(rel[bucket].transpose(0, 2, 1))
    maskc = np.where((dist >= 0) & (dist < 128), 0.0, NEG).astype(np.float32)
    chanp = np.zeros((128, 48), np.float32)
    for l in range(2):
        for i in range(2):
            sl = slice(i * 128, (i + 1) * 128)
            cb = 24 * l + i * 4
            chanp[:, cb:cb + 3] = f(inp["sc_conv_w"])[l][:, sl].T
            chanp[:, cb + 3] = f(inp["sc_conv_b"])[l][sl]
            cb = 24 * l + 8 + i * 5
            chanp[:, cb:cb + 4] = f(inp["rg_conv_w"])[l][:, sl].T
            chanp[:, cb + 4] = f(inp["rg_conv_b"])[l][sl]
            cg = 24 * l + 18 + i * 3
            chanp[:, cg] = f(inp["rg_b_a"])[l][sl]
            chanp[:, cg + 1] = f(inp["rg_b_x"])[l][sl]
            chanp[:, cg + 2] = f(inp["rg_lambda"])[l][sl]
    rgw = np.ascontiguousarray(np.stack([f(inp["rg_w_a"]), f(inp["rg_w_x"])], 1))
    shared = {
        "w_inp": w_inp, "w_out": f(inp["w_out"]), "xa_wq": f(inp["xa_wq"]), "xa_wk": f(inp["xa_wk"]),
        "xa_wv": f(inp["xa_wv"]), "xa_wo": f(inp["xa_wo"]),
        "dense_wg": f(inp["dense_wg"]), "dense_wu": f(inp["dense_wu"]), "dense_wd": f(inp["dense_wd"]),
        "moe_wg": f(inp["moe_wg"])[0], "moe_wu": f(inp["moe_wu"])[0], "moe_wd": f(inp["moe_wd"])[0],
        "router": f(inp["moe_router"])[0], "gains": np.ascontiguousarray(gains),
        "biasg": biasg, "maskc": maskc, "sinks": f(inp["attn_sinks"]).reshape(1, 16),
        "chanp": chanp, "rgw": rgw, "ident": np.eye(128, dtype=np.float32),
    }
    return shared


def kernel(**inputs):
    shared = prepare_shared(inputs)
    x = np.asarray(inputs["x"], dtype=np.float32)
    mem = np.asarray(inputs["mem"], dtype=np.float32)
    nc = bass.Bass("TRN2", target_bir_lowering=False)
    build_program(nc, SEQ // T)
    in_maps = []
    for c in range(NCORES):
        m = dict(shared)
        m["x"] = np.ascontiguousarray(x[c])
        m["mem"] = np.ascontiguousarray(mem[c])
        in_maps.append(m)
    res = run_bass_kernel_spmd(nc, in_maps, core_ids=list(range(NCORES)))
    return np.stack([np.asarray(r["y"], dtype=np.float32) for r in res.results], 0)
```
